# Optimizing a Trainium2 kernel written in Bass

```python
import math
import numpy as np
import jax, jax.numpy as jnp
from jax import lax

D_MODEL = 2048
BATCH = 2
SEQ = 8192
DEPTH = 4

GRID_W = 64
CTX_LEN = 256
HEAD_DIM = 128
DN_HEADS = 8
DN_WIDTH = DN_HEADS * HEAD_DIM
CONV_K = 5
CHUNK = 64
DT_MIN = 0.001
DT_MAX = 0.1
ATTN_Q_HEADS = 4
ATTN_KV_HEADS = 2
ATTN_WIDTH = ATTN_Q_HEADS * HEAD_DIM
ATTN_KV_WIDTH = 2 * ATTN_KV_HEADS * HEAD_DIM
Q_BLOCK = 128
ROPE_THETA = 10000.0
FNET_GROUPS = 4
FNET_GROUP_DIM = 128
FNET_WIDTH = FNET_GROUPS * FNET_GROUP_DIM
MIX_WIDTH = DN_WIDTH + ATTN_WIDTH + FNET_WIDTH
IN_SPLITS = (3 * DN_WIDTH, DN_WIDTH, 2 * DN_HEADS, 2 * DN_HEADS, ATTN_WIDTH, ATTN_KV_WIDTH, FNET_WIDTH)
IN_COLS = 4 * DN_WIDTH + 4 * DN_HEADS + ATTN_WIDTH + ATTN_KV_WIDTH + FNET_WIDTH
D_FF = 5632
N_EXPERTS = 8
TOP_K = 2
N_DENSE = (DEPTH + 1) // 2
N_MOE = DEPTH // 2
DEEPNORM_ALPHA = (2 * DEPTH) ** 0.25
DEEPNORM_BETA = (8 * DEPTH) ** -0.25
EPS = 1e-6

kernel_name = "hybrid_dit_deltanet_axialgqa_fnet_moe"


def _layer_norm(x, gain=None, bias=None):
    xf = x.astype(jnp.float32)
    mu = jnp.mean(xf, axis=-1, keepdims=True)
    var = jnp.mean(jnp.square(xf - mu), axis=-1, keepdims=True)
    y = (xf - mu) * lax.rsqrt(var + EPS)
    if gain is not None:
        y = y * gain.astype(jnp.float32) + bias.astype(jnp.float32)
    return y.astype(x.dtype)


def _modulate(x, shift, scale):
    return _layer_norm(x) * (1.0 + scale) + shift


def _rms_norm(x, w):
    xf = x.astype(jnp.float32)
    y = xf * lax.rsqrt(jnp.mean(jnp.square(xf), axis=-1, keepdims=True) + EPS)
    return (y * w.astype(jnp.float32)).astype(x.dtype)


def _l2norm(x):
    return x * lax.rsqrt(jnp.sum(jnp.square(x), axis=-1, keepdims=True) + EPS)


def _split_in(p):
    return jnp.split(p, np.cumsum(IN_SPLITS)[:-1].tolist(), axis=-1)


def _short_conv(u, w):
    pad = CONV_K // 2
    l = u.shape[1]
    up = jnp.pad(u, ((0, 0), (pad, pad), (0, 0)))
    y = up[:, 0:l] * w[0]
    for tap in range(1, CONV_K):
        y = y + up[:, tap:tap + l] * w[tap]
    return jax.nn.silu(y)


def _gated_delta_chunked(q, k, v, g, beta, state0):
    b, h, l, dk = q.shape
    dv = v.shape[-1]
    n = l // CHUNK
    q = q * dk ** -0.5
    k_beta = k * beta[..., None]
    v_beta = v * beta[..., None]
    rs = lambda t: t.reshape(b, h, n, CHUNK, t.shape[-1])
    q, k, k_beta, v_beta = rs(q), rs(k), rs(k_beta), rs(v_beta)
    g = jnp.cumsum(g.reshape(b, h, n, CHUNK), axis=-1)
    tril = jnp.tril(jnp.ones((CHUNK, CHUNK), bool))
    strict = jnp.tril(jnp.ones((CHUNK, CHUNK), bool), -1)
    decay = jnp.exp(jnp.where(tril, g[..., :, None] - g[..., None, :], -jnp.inf))
    lmat = jnp.where(strict, jnp.einsum('bhnid,bhnjd->bhnij', k_beta, k) * decay, 0.0)
    rhs = jnp.concatenate([v_beta, k_beta * jnp.exp(g)[..., None]], axis=-1)
    sol = lax.linalg.triangular_solve(lmat, rhs, left_side=True, lower=True, unit_diagonal=True)
    u, w = sol[..., :dv], sol[..., dv:]
    qk = jnp.where(tril, jnp.einsum('bhnid,bhnjd->bhnij', q, k) * decay, 0.0)

    def step(s, inp):
        q_c, k_c, u_c, w_c, g_c, qk_c = inp
        v_new = u_c - jnp.einsum('bhcd,bhde->bhce', w_c, s)
        o = (jnp.einsum('bhcd,bhde->bhce', q_c * jnp.exp(g_c)[..., None], s)
             + jnp.einsum('bhij,bhje->bhie', qk_c, v_new))
        g_last = g_c[..., -1]
        s = (s * jnp.exp(g_last)[..., None, None]
             + jnp.einsum('bhcd,bhce->bhde', k_c * jnp.exp(g_last[..., None] - g_c)[..., None], v_new))
        return s, o

    xs = tuple(jnp.moveaxis(t, 2, 0) for t in (q, k, u, w, g, qk))
    s_final, o = lax.scan(step, state0, xs)
    o = jnp.moveaxis(o, 0, 2).reshape(b, h, l, dv)
    return o, s_final


def _deltanet_prep(qkv_raw, beta_raw, a_raw, conv_w, a_log, dt_bias):
    b, l, _ = qkv_raw.shape
    qkv = _short_conv(qkv_raw, conv_w).astype(jnp.float32).reshape(b, l, 3, DN_HEADS, HEAD_DIM)
    qkv = jnp.transpose(qkv, (2, 0, 3, 1, 4))
    q, k, v = _l2norm(qkv[0]), _l2norm(qkv[1]), qkv[2]
    beta = jnp.transpose(jax.nn.sigmoid(beta_raw.astype(jnp.float32)).reshape(b, l, 2, DN_HEADS), (2, 0, 3, 1))
    dt = jax.nn.softplus(a_raw.astype(jnp.float32).reshape(b, l, 2, DN_HEADS) + dt_bias.astype(jnp.float32))
    g = -jnp.exp(a_log.astype(jnp.float32))[:, None, :, None] * jnp.transpose(dt, (2, 0, 3, 1))
    return q, k, v, beta, g


def _gated_rms_out(o, gate_raw, norm_w):
    b, h, l, d = o.shape
    o = jnp.transpose(o, (0, 2, 1, 3))
    y = o * lax.rsqrt(jnp.mean(jnp.square(o), axis=-1, keepdims=True) + EPS) * norm_w.astype(jnp.float32)
    y = y * jax.nn.silu(gate_raw.astype(jnp.float32).reshape(b, l, h, d))
    return y.reshape(b, l, h * d).astype(gate_raw.dtype)


def _deltanet(lat, con, conv_w, a_log, dt_bias, norm_w, need_ctx):
    qc, kc, vc, bc, gc = _deltanet_prep(con[0], con[2], con[3], conv_w, a_log, dt_bias)
    ql, kl, vl, bl, gl = _deltanet_prep(lat[0], lat[2], lat[3], conv_w, a_log, dt_bias)
    s0 = jnp.zeros(qc.shape[:2] + (HEAD_DIM, HEAD_DIM), jnp.float32)
    flip = lambda t: jnp.flip(t, axis=2)
    oc_f, s_f = _gated_delta_chunked(qc, kc, vc, gc[0], bc[0], s0)
    ol_f, _ = _gated_delta_chunked(ql, kl, vl, gl[0], bl[0], s_f)
    oc_b, s_b = _gated_delta_chunked(flip(qc), flip(kc), flip(vc), flip(gc[1]), flip(bc[1]), s0)
    ol_b, _ = _gated_delta_chunked(flip(ql), flip(kl), flip(vl), flip(gl[1]), flip(bl[1]), s_b)
    out_l = _gated_rms_out(ol_f + flip(ol_b), lat[1], norm_w)
    out_c = _gated_rms_out(oc_f + flip(oc_b), con[1], norm_w) if need_ctx else None
    return out_l, out_c


def _axial_rope_tables(rows):
    axis_dim = HEAD_DIM // 2
    inv = ROPE_THETA ** (-jnp.arange(0, axis_dim, 2, dtype=jnp.float32) / axis_dim)
    row = jnp.repeat(jnp.arange(rows, dtype=jnp.float32), GRID_W)
    col = jnp.tile(jnp.arange(GRID_W, dtype=jnp.float32), rows)
    ang = jnp.concatenate([row[:, None] * inv, col[:, None] * inv], axis=-1)
    return jnp.cos(ang), jnp.sin(ang)


def _apply_axial_rope(x, cos, sin):
    b, l, h, d = x.shape
    xa = x.astype(jnp.float32).reshape(b, l, h, 2, 2, d // 4)
    x1, x2 = xa[..., 0, :], xa[..., 1, :]
    cs = cos.reshape(l, 1, 2, d // 4)
    sn = sin.reshape(l, 1, 2, d // 4)
    out = jnp.stack([x1 * cs - x2 * sn, x1 * sn + x2 * cs], axis=-2)
    return out.reshape(b, l, h, d).astype(x.dtype)


def _attn_prep(q_raw, kv_raw, q_norm_w, k_norm_w):
    b, l, _ = q_raw.shape
    q = _rms_norm(q_raw.reshape(b, l, ATTN_Q_HEADS, HEAD_DIM), q_norm_w)
    kv = kv_raw.reshape(b, l, 2, ATTN_KV_HEADS, HEAD_DIM)
    k = _rms_norm(kv[:, :, 0], k_norm_w)
    return q, k, kv[:, :, 1]


def _attend_blocks(q, k, v):
    b, l, hq, d = q.shape
    grp = hq // ATTN_KV_HEADS
    nb = l // Q_BLOCK
    qb = jnp.moveaxis(q.reshape(b, nb, Q_BLOCK, ATTN_KV_HEADS, grp, d), 1, 0)
    scale = d ** -0.5

    def one_block(qi):
        s = jnp.einsum('bqhgd,bkhd->bhgqk', qi, k).astype(jnp.float32) * scale
        p = jax.nn.softmax(s, axis=-1).astype(v.dtype)
        return jnp.einsum('bhgqk,bkhd->bqhgd', p, v)

    o = lax.map(one_block, qb)
    return jnp.moveaxis(o, 0, 1).reshape(b, l, hq, d)


def _axial_gqa(lat, con, q_norm_w, k_norm_w, rope_cos, rope_sin, need_ctx):
    ql, kl, vl = _attn_prep(lat[0], lat[1], q_norm_w, k_norm_w)
    qc, kc, vc = _attn_prep(con[0], con[1], q_norm_w, k_norm_w)
    ql = _apply_axial_rope(ql, rope_cos, rope_sin)
    kl = _apply_axial_rope(kl, rope_cos, rope_sin)
    k_all = jnp.concatenate([kl, kc], axis=1)
    v_all = jnp.concatenate([vl, vc], axis=1)
    b, l = ql.shape[:2]
    out_l = _attend_blocks(ql, k_all, v_all).reshape(b, l, ATTN_WIDTH)
    out_c = _attend_blocks(qc, kc, vc).reshape(b, qc.shape[1], ATTN_WIDTH) if need_ctx else None
    return out_l, out_c


def _fourier_mix(u, w):
    b, l, _ = u.shape
    ug = u.astype(jnp.float32).reshape(b, l, FNET_GROUPS, FNET_GROUP_DIM)
    f = jnp.fft.fft2(ug, axes=(1, 3), norm="ortho").real
    return f.reshape(b, l, FNET_WIDTH).astype(u.dtype) @ w


def _token_mixers(pl, pc, conv_w, a_log, dt_bias, dn_norm_w, q_norm_w, k_norm_w, fnet_w,
                  rope_cos, rope_sin, need_ctx):
    lat = _split_in(pl)
    con = _split_in(pc)
    dl, dc = _deltanet(lat[0:4], con[0:4], conv_w, a_log, dt_bias, dn_norm_w, need_ctx)
    al, ac = _axial_gqa(lat[4:6], con[4:6], q_norm_w, k_norm_w, rope_cos, rope_sin, need_ctx)
    ml = jnp.concatenate([dl, al, _fourier_mix(lat[6], fnet_w)], axis=-1)
    mc = jnp.concatenate([dc, ac, _fourier_mix(con[6], fnet_w)], axis=-1) if need_ctx else None
    return ml, mc


def _swiglu(h, wg, wu, wd):
    return (jax.nn.silu(h @ wg) * (h @ wu)) @ wd


def _moe_swiglu(h, router, wg, wu, wd):
    logits = (h @ router).astype(jnp.float32)
    top_v, top_i = lax.top_k(logits, TOP_K)
    gates = jax.nn.softmax(top_v, axis=-1)
    combine = jnp.sum(jax.nn.one_hot(top_i, N_EXPERTS, dtype=jnp.float32) * gates[..., None], axis=-2)
    out = combine[..., 0:1].astype(h.dtype) * _swiglu(h, wg[0], wu[0], wd[0])
    for e in range(1, N_EXPERTS):
        out = out + combine[..., e:e + 1].astype(h.dtype) * _swiglu(h, wg[e], wu[e], wd[e])
    return out


def setup_inputs(seed: int = 0) -> dict:
    key = jax.random.key(seed)
    ks = jax.random.split(key, 26)
    f32 = jnp.float32
    nrm = lambda k, shape, s: jax.random.normal(k, shape, f32) * s
    d = D_MODEL
    dt = jnp.exp(jax.random.uniform(ks[7], (DEPTH, 2, DN_HEADS), f32, math.log(DT_MIN), math.log(DT_MAX)))
    return {
        "x": nrm(ks[0], (BATCH, SEQ, d), 1.0),
        "c": nrm(ks[1], (BATCH, d), 1.0),
        "ctx": nrm(ks[2], (BATCH, CTX_LEN, d), 1.0),
        "c_ctx": nrm(ks[3], (d,), 1.0),
        "w_mod": nrm(ks[4], (DEPTH, d, 6 * d), d ** -0.5),
        "b_mod": nrm(ks[5], (DEPTH, 6 * d), 0.02),
        "w_in": nrm(ks[6], (DEPTH, d, IN_COLS), d ** -0.5),
        "dn_conv": nrm(ks[8], (DEPTH, CONV_K, 3 * DN_WIDTH), CONV_K ** -0.5),
        "dn_a_log": jnp.log(jax.random.uniform(ks[9], (DEPTH, 2, DN_HEADS), f32, 1.0, 16.0)),
        "dn_dt_bias": dt + jnp.log(-jnp.expm1(-dt)),
        "dn_norm": 1.0 + nrm(ks[10], (DEPTH, HEAD_DIM), 0.02),
        "attn_q_norm": 1.0 + nrm(ks[11], (DEPTH, HEAD_DIM), 0.02),
        "attn_k_norm": 1.0 + nrm(ks[12], (DEPTH, HEAD_DIM), 0.02),
        "fnet_w": nrm(ks[13], (DEPTH, FNET_WIDTH, FNET_WIDTH), FNET_WIDTH ** -0.5),
        "w_out": nrm(ks[14], (DEPTH, MIX_WIDTH, d), MIX_WIDTH ** -0.5 * DEEPNORM_BETA),
        "ln1_g": 1.0 + nrm(ks[15], (DEPTH, d), 0.02),
        "ln1_b": nrm(ks[16], (DEPTH, d), 0.02),
        "ln2_g": 1.0 + nrm(ks[17], (DEPTH, d), 0.02),
        "ln2_b": nrm(ks[18], (DEPTH, d), 0.02),
        "ffn_w_gate": nrm(ks[19], (N_DENSE, d, D_FF), d ** -0.5),
        "ffn_w_up": nrm(ks[20], (N_DENSE, d, D_FF), d ** -0.5),
        "ffn_w_down": nrm(ks[21], (N_DENSE, D_FF, d), D_FF ** -0.5 * DEEPNORM_BETA),
        "router": nrm(ks[22], (N_MOE, d, N_EXPERTS), d ** -0.5),
        "moe_w_gate": nrm(ks[23], (N_MOE, N_EXPERTS, d, D_FF), d ** -0.5),
        "moe_w_up": nrm(ks[24], (N_MOE, N_EXPERTS, d, D_FF), d ** -0.5),
        "moe_w_down": nrm(ks[25], (N_MOE, N_EXPERTS, D_FF, d), D_FF ** -0.5 * DEEPNORM_BETA),
    }


def reference(x, c, ctx, c_ctx, w_mod, b_mod, w_in, dn_conv, dn_a_log, dn_dt_bias, dn_norm,
              attn_q_norm, attn_k_norm, fnet_w, w_out, ln1_g, ln1_b, ln2_g, ln2_b,
              ffn_w_gate, ffn_w_up, ffn_w_down, router, moe_w_gate, moe_w_up, moe_w_down):
    rows = x.shape[1] // GRID_W
    rope_cos, rope_sin = _axial_rope_tables(rows)
    silu_c = jax.nn.silu(c)
    silu_cc = jax.nn.silu(c_ctx)
    xl, xc = x, ctx
    for layer in range(DEPTH):
        need_ctx = layer < DEPTH - 1
        mod_l = jnp.split((silu_c @ w_mod[layer] + b_mod[layer])[:, None, :], 6, axis=-1)
        mod_c = jnp.split(silu_cc @ w_mod[layer] + b_mod[layer], 6, axis=-1)
        pl = _modulate(xl, mod_l[0], mod_l[1]) @ w_in[layer]
        pc = _modulate(xc, mod_c[0], mod_c[1]) @ w_in[layer]
        ml, mc = _token_mixers(pl, pc, dn_conv[layer], dn_a_log[layer], dn_dt_bias[layer], dn_norm[layer],
                               attn_q_norm[layer], attn_k_norm[layer], fnet_w[layer],
                               rope_cos, rope_sin, need_ctx)
        xl = _layer_norm(DEEPNORM_ALPHA * xl + mod_l[2] * (ml @ w_out[layer]), ln1_g[layer], ln1_b[layer])
        if need_ctx:
            xc = _layer_norm(DEEPNORM_ALPHA * xc + mod_c[2] * (mc @ w_out[layer]), ln1_g[layer], ln1_b[layer])
        i = layer // 2
        if layer % 2 == 0:
            channel_mix = lambda h: _swiglu(h, ffn_w_gate[i], ffn_w_up[i], ffn_w_down[i])
        else:
            channel_mix = lambda h: _moe_swiglu(h, router[i], moe_w_gate[i], moe_w_up[i], moe_w_down[i])
        yl = channel_mix(_modulate(xl, mod_l[3], mod_l[4]))
        xl = _layer_norm(DEEPNORM_ALPHA * xl + mod_l[5] * yl, ln2_g[layer], ln2_b[layer])
        if need_ctx:
            yc = channel_mix(_modulate(xc, mod_c[3], mod_c[4]))
            xc = _layer_norm(DEEPNORM_ALPHA * xc + mod_c[5] * yc, ln2_g[layer], ln2_b[layer])
    return xl
```

```python
import contextlib
import math
import numpy as np
import ml_dtypes
import concourse.bass as bass
import concourse.mybir as mybir
from concourse.bass_utils import run_bass_kernel_spmd

F32 = mybir.dt.float32
BF16 = mybir.dt.bfloat16
ALU = mybir.AluOpType
AF = mybir.ActivationFunctionType
ENGS = ("pe", "act", "dve", "pool", "sp")
NPBF = ml_dtypes.bfloat16


class TT:
    def __init__(self, h, name):
        self.h = h
        self.name = name
        self.w = None
        self.r = {}
        self.dsem = None

    def __getitem__(self, idx):
        return self.h[idx]


class _Rec:
    def __init__(self):
        self.call = None

    def __getattr__(self, name):
        def f(*a, **k):
            self.call = (name, a, k)
            return self
        return f


class Prog:
    def __init__(self):
        self.nc = bass.Bass("TRN2", target_bir_lowering=False)
        self.es = contextlib.ExitStack()
        self.stacks = [self.es]
        self.scope_tiles = [[]]
        self.ops = {e: [] for e in ENGS}
        self.sem = {}
        self.cnt = {}
        self.seen = {e: {} for e in ENGS}
        for e in ENGS:
            self._mksem("E_" + e)
        self.pool_free = []
        self.ndsem = 0
        self.uid = 0

    def _mksem(self, key):
        self.sem[key] = self.es.enter_context(self.nc.semaphore(key))
        self.cnt[key] = 0
        assert len(self.sem) <= 100, "too many semaphores"

    def _getdsem(self):
        if self.pool_free:
            return self.pool_free.pop()
        k = "D%d" % self.ndsem
        self.ndsem += 1
        self._mksem(k)
        return k

    def name(self, n):
        self.uid += 1
        return "%s_%d" % (n, self.uid)

    def sb(self, name, shape, dt):
        h = self.stacks[-1].enter_context(self.nc.sbuf_tensor(self.name(name), list(shape), dt))
        t = TT(h, name)
        self.scope_tiles[-1].append(t)
        return t

    def ps(self, name, shape, dt=F32):
        h = self.stacks[-1].enter_context(self.nc.psum_tensor(self.name(name), list(shape), dt))
        t = TT(h, name)
        self.scope_tiles[-1].append(t)
        return t

    def dram(self, name, shape, dt, kind="Internal"):
        h = self.nc.dram_tensor(name, list(shape), dt, kind=kind)
        return TT(h, name)

    @contextlib.contextmanager
    def scope(self):
        st = contextlib.ExitStack()
        self.stacks.append(st)
        self.scope_tiles.append([])
        try:
            yield
        finally:
            self.barrier()
            for t in self.scope_tiles.pop():
                if t.dsem is not None:
                    self.pool_free.append(t.dsem)
                    t.dsem = None
            self.stacks.pop()
            st.close()

    def barrier(self):
        for e in ENGS:
            for k, c in self.cnt.items():
                if c > 0 and self.seen[e].get(k, 0) < c:
                    self.seen[e][k] = c
                    if k == "E_pe" and e == "pe":
                        continue
                    sem = self.sem[k]
                    self.ops[e].append(lambda en, sem=sem, c=c: en.wait_ge(sem, c))

    def _deps(self, eng, reads, writes):
        deps = {}

        def add(k, c):
            if deps.get(k, 0) < c:
                deps[k] = c
        for t in reads:
            if t.w is not None:
                add(*t.w)
        for t in writes:
            if t.w is not None:
                add(*t.w)
            for k, c in t.r.items():
                add(k, c)
        own = "E_" + eng
        for k, c in deps.items():
            if k == own and eng == "pe":
                continue
            if self.seen[eng].get(k, 0) >= c:
                continue
            self.seen[eng][k] = c
            sem = self.sem[k]
            self.ops[eng].append(lambda e, sem=sem, c=c: e.wait_ge(sem, c))

    def _mark(self, tok, reads, writes):
        k, c = tok
        for t in reads:
            if t.r.get(k, 0) < c:
                t.r[k] = c
        for t in writes:
            t.w = tok
            t.r = {}

    def op(self, eng, fn, reads=(), writes=()):
        self._deps(eng, reads, writes)
        k = "E_" + eng
        self.cnt[k] += 1
        sem = self.sem[k]
        rec = _Rec()
        fn(rec)
        name, a, kw = rec.call
        self.ops[eng].append(lambda e, name=name, a=a, kw=kw, sem=sem: getattr(e, name)(*a, **kw).then_inc(sem, 1))
        self._mark((k, self.cnt[k]), reads, writes)

    def dma(self, q, out_ap, in_ap, reads=(), writes=(), own=None):
        self._deps(q, reads, writes)
        t = own if own is not None else (writes[0] if writes else reads[0])
        if t.dsem is None:
            t.dsem = self._getdsem()
        k = t.dsem
        self.cnt[k] += 16
        sem = self.sem[k]
        self.ops[q].append(lambda e, o=out_ap, i=in_ap, sem=sem: e.dma_start(out=o, in_=i).then_inc(sem, 16))
        self._mark((k, self.cnt[k]), reads, writes)

    def allgather(self, src, dst, groups, src_ap=None, dst_ap=None):
        self._deps("pool", [src], [dst])
        if dst.dsem is None:
            dst.dsem = self._getdsem()
        k = dst.dsem
        self.cnt[k] += 1
        sem = self.sem[k]
        si = (src.h.ap() if src_ap is None else src_ap).opt()
        di = (dst.h.ap() if dst_ap is None else dst_ap).opt()
        self.ops["pool"].append(lambda e: e.collective_compute(
            "AllGather", ALU.bypass, replica_groups=groups, ins=[si], outs=[di]).then_inc(sem))
        self._mark((k, self.cnt[k]), [src], [dst])

    def finish(self, final_tiles):
        self._deps("sp", list(final_tiles), [])
        self.barrier()
        nc = self.nc
        with nc.Block() as block:
            @block.tensor
            def _(e):
                for f in self.ops["pe"]:
                    f(e)

            @block.scalar
            def _(e):
                for f in self.ops["act"]:
                    f(e)

            @block.vector
            def _(e):
                for f in self.ops["dve"]:
                    f(e)

            @block.gpsimd
            def _(e):
                for f in self.ops["pool"]:
                    f(e)

            @block.sync
            def _(e):
                for f in self.ops["sp"]:
                    f(e)
        self.es.close()
        return nc


class Cfg:
    def __init__(self, D=2048, L=8192, CTX=256, GW=64, DFF=5632, DEPTH=4, E=8):
        self.D, self.L, self.CTX, self.GW, self.DFF, self.DEPTH, self.E = D, L, CTX, GW, DFF, DEPTH, E
        self.KC = D // 128
        self.FC = DFF // 128
        self.NL = L // 4
        self.NCX = CTX // 4
        self.TOK = self.NL + self.NCX
        self.S = CTX + L
        self.TW = min(512, self.NL)
        self.NDENSE = (DEPTH + 1) // 2
        self.NMOE = DEPTH // 2
        self.alpha = (2 * DEPTH) ** 0.25
        self.ttiles = [(i * self.TW, self.TW, False) for i in range(self.NL // self.TW)] + [(self.NL, self.NCX, True)]
        self.NCH = 13


EPS = 1e-6
import os
DN_STOP = os.environ.get("DN_STOP", "")
P4_STOP = os.environ.get("P4_STOP", "")


def blk(W):
    K, N = W.shape
    return np.ascontiguousarray(W.reshape(K // 128, 128, N // 128, 128).transpose(2, 1, 0, 3).reshape(N, K))


def fm(v):
    sh = v.shape
    n = sh[-1] // 128
    x = v.reshape(sh[:-1] + (n, 128))
    return np.ascontiguousarray(np.moveaxis(x, -1, 0))


def own_cols(j):
    cols = []
    h0, h1 = 2 * j, 2 * j + 1
    for base in (0, 1024, 2048, 3072):
        for h in (h0, h1):
            cols += list(range(base + h * 128, base + (h + 1) * 128))
    cols += list(range(4128 + j * 128, 4128 + (j + 1) * 128))
    kv = j // 2
    cols += list(range(4640 + kv * 128, 4640 + (kv + 1) * 128))
    cols += list(range(4640 + 256 + kv * 128, 4640 + 256 + (kv + 1) * 128))
    cols += list(range(5152 + j * 128, 5152 + (j + 1) * 128))
    misc = [4096 + h0, 4096 + h1, 4096 + 8 + h0, 4096 + 8 + h1,
            4112 + h0, 4112 + h1, 4112 + 8 + h0, 4112 + 8 + h1]
    cols += misc + [-1] * 120
    return np.array(cols)


def wout_perm():
    rows = []
    for i in range(4):
        for r in range(4):
            base = [(2 * r) * 128, (2 * r + 1) * 128, 1024 + r * 128, 1536 + r * 128][i]
            rows += list(range(base, base + 128))
    return np.array(rows)


def const_tables(cfg):
    t = {}
    t["ident"] = np.eye(128, dtype=np.float32)
    i = np.arange(128)
    NEG = -30000.0
    m = np.zeros((128, 8, 128), np.float32)
    ii, jj = np.meshgrid(i, i, indexing="ij")
    m[:, 0, :] = np.where(jj < ii, 0, NEG)
    m[:, 1, :] = np.where(jj >= ii, 0, NEG)
    m[:, 2, :] = np.where(jj > ii, 0, NEG)
    m[:, 3, :] = np.where(jj > ii, 0, NEG)
    m[:, 4, :] = np.where(jj <= ii, 0, NEG)
    m[:, 5, :] = np.where(jj < ii, 0, NEG)
    t["negmask"] = m
    tri = np.zeros((128, 2, 128), np.float32)
    tri[:, 0, :] = (ii <= jj)
    tri[:, 1, :] = (ii >= jj)
    t["tri"] = tri
    bmk = np.zeros((128, 7, 128), np.float32)
    bmk[:, 0, :] = (ii // 16 == jj // 16)
    for m_, s_ in enumerate((16, 32, 64)):
        low = (ii // (2 * s_) == jj // (2 * s_)) & (ii % (2 * s_) >= s_) & (jj % (2 * s_) < s_)
        bmk[:, 1 + m_, :] = low
        bmk[:, 4 + m_, :] = low.T
    t["blkmask"] = bmk
    R = np.zeros((128, 128), np.float32)
    for mm in range(128):
        if (mm % 64) < 32:
            R[mm + 32, mm] = -1.0
        else:
            R[mm - 32, mm] = 1.0
    t["roperot"] = R
    sel = np.zeros((8, 8, 128), np.float32)
    for k in range(8):
        sel[k, k, :] = 1.0
    t["sel"] = sel
    rows = cfg.L // cfg.GW
    inv = 10000.0 ** (-np.arange(0, 64, 2, dtype=np.float64) / 64)
    row = np.repeat(np.arange(rows, dtype=np.float64), cfg.GW)
    col = np.tile(np.arange(cfg.GW, dtype=np.float64), rows)
    ang = np.concatenate([row[:, None] * inv, col[:, None] * inv], axis=-1)
    pidx = (i // 64) * 32 + (i % 32)
    t["ropecos"] = np.ascontiguousarray(np.cos(ang)[:, pidx].T).astype(np.float32)
    t["ropesin"] = np.ascontiguousarray(np.sin(ang)[:, pidx].T).astype(np.float32)
    cc = np.outer(i, i).astype(np.float64) * (2 * np.pi / 128)
    t["dftc"] = np.concatenate([np.cos(cc), -np.sin(cc)], axis=1).astype(NPBF)
    for nm, n in (("L", cfg.L), ("X", cfg.CTX)):
        s = np.arange(n)
        a = (np.outer(s, s) % n).astype(np.float64) * (2 * np.pi / n)
        sc = 1.0 / math.sqrt(n * 128.0)
        t["cos" + nm] = (np.cos(a) * sc).astype(NPBF)
        t["sin" + nm] = (np.sin(a) * sc).astype(NPBF)
    return t


def make_in_maps(cfg, inp):
    D, KC, DEPTH = cfg.D, cfg.KC, cfg.DEPTH
    tabs = const_tables(cfg)
    f32 = np.float32
    maps = []
    wperm = wout_perm()
    wmod_b = [blk(np.asarray(inp["w_mod"][l], f32)) for l in range(DEPTH)]
    wout_b = [blk(np.asarray(inp["w_out"][l], f32)[wperm]) for l in range(DEPTH)]
    fnw_b = [blk(np.asarray(inp["fnet_w"][l], f32)) for l in range(DEPTH)]
    fg_b = [blk(np.asarray(inp["ffn_w_gate"][i], f32)) for i in range(cfg.NDENSE)]
    fu_b = [blk(np.asarray(inp["ffn_w_up"][i], f32)) for i in range(cfg.NDENSE)]
    fd_b = [blk(np.asarray(inp["ffn_w_down"][i], f32)) for i in range(cfg.NDENSE)]
    mg_b = [[blk(np.asarray(inp["moe_w_gate"][i, e], f32)) for e in range(cfg.E)] for i in range(cfg.NMOE)]
    mu_b = [[blk(np.asarray(inp["moe_w_up"][i, e], f32)) for e in range(cfg.E)] for i in range(cfg.NMOE)]
    md_b = [[blk(np.asarray(inp["moe_w_down"][i, e], f32)) for e in range(cfg.E)] for i in range(cfg.NMOE)]

    def shard(a, c):
        w = a.shape[1] // 4
        pos = c % 4
        return np.ascontiguousarray(a[:, pos * w:(pos + 1) * w])

    for c in range(8):
        b, j = c // 4, c % 4
        m = {}
        xl = np.asarray(inp["x"][b, j * cfg.NL:(j + 1) * cfg.NL, :], f32)
        xc = np.asarray(inp["ctx"][b, j * cfg.NCX:(j + 1) * cfg.NCX, :], f32)
        m["xT"] = np.ascontiguousarray(np.concatenate([xl, xc], axis=0).T)
        m["cvec"] = np.ascontiguousarray(np.stack([fm(np.asarray(inp["c"][b], f32)),
                                                   fm(np.asarray(inp["c_ctx"], f32))], axis=-1))
        m["bmod"] = fm(np.asarray(inp["b_mod"], f32))
        m["lnp"] = np.ascontiguousarray(np.stack([fm(np.asarray(inp[k], f32)) for k in
                                                  ("ln1_g", "ln1_b", "ln2_g", "ln2_b")], axis=2))
        oc = own_cols(j)
        wi = np.asarray(inp["w_in"], f32)
        wsel = np.where(oc[None, None, :] >= 0, wi[:, :, np.maximum(oc, 0)], 0.0).astype(f32)
        m["win"] = np.stack([blk(wsel[l]) for l in range(DEPTH)])
        cw = np.asarray(inp["dn_conv"], f32)[:, :, oc[:768]]
        m["convw"] = np.ascontiguousarray(cw.reshape(DEPTH, 5, 6, 128).transpose(3, 0, 2, 1))
        hs = [(0, 2 * j), (0, 2 * j + 1), (1, 2 * j), (1, 2 * j + 1)]
        al = np.array([[inp["dn_a_log"][l][d][h] for (d, h) in hs] for l in range(DEPTH)], f32)
        db = np.array([[inp["dn_dt_bias"][l][d][h] for (d, h) in hs] for l in range(DEPTH)], f32)
        m["dnsc"] = np.ascontiguousarray(np.broadcast_to(np.stack([al, db], axis=1)[None], (128, DEPTH, 2, 4))).astype(f32)
        m["normw"] = np.ascontiguousarray(np.stack([np.asarray(inp[k], f32).T for k in
                                                    ("dn_norm", "attn_q_norm", "attn_k_norm")], axis=2))
        m["router"] = np.ascontiguousarray(np.asarray(inp["router"], f32).reshape(cfg.NMOE, KC, 128, cfg.E)
                                           .transpose(2, 0, 1, 3))
        for k, v in tabs.items():
            if k in ("cosL", "sinL"):
                m[k] = shard(v, c)
            else:
                m[k] = v
        m["wmod"] = np.stack([shard(a, c) for a in wmod_b])
        m["wout"] = np.stack([shard(a, c) for a in wout_b])
        m["fnw"] = np.stack([shard(a, c) for a in fnw_b])
        m["ffg"] = np.stack([shard(a, c) for a in fg_b])
        m["ffu"] = np.stack([shard(a, c) for a in fu_b])
        m["ffd"] = np.stack([shard(a, c) for a in fd_b])
        if cfg.NMOE:
            m["mog"] = np.stack([np.stack([shard(a, c) for a in le]) for le in mg_b])
            m["mou"] = np.stack([np.stack([shard(a, c) for a in le]) for le in mu_b])
            m["mod"] = np.stack([np.stack([shard(a, c) for a in le]) for le in md_b])
        maps.append(m)
    return maps


def build(cfg, debug=False, nlayers=None, stop_after=None):
    P = Prog()
    D, KC, FC, TOK, S, NL, NCX, CTX, L, TW, E = (cfg.D, cfg.KC, cfg.FC, cfg.TOK, cfg.S, cfg.NL, cfg.NCX,
                                                 cfg.CTX, cfg.L, cfg.TW, cfg.E)
    DEPTH = cfg.DEPTH if nlayers is None else nlayers
    PAIRS = [[0, 4], [1, 5], [2, 6], [3, 7]]
    GRP = [[0, 1, 2, 3], [4, 5, 6, 7]]

    def inp(name, shape, dt=F32):
        return P.dram(name, shape, dt, kind="ExternalInput")

    xT = inp("xT", [D, TOK])
    cvec = inp("cvec", [128, KC, 2])
    bmod = inp("bmod", [128, cfg.DEPTH, 6 * KC])
    lnp = inp("lnp", [128, cfg.DEPTH, 4, KC])
    win = inp("win", [cfg.DEPTH, 13 * 128, D])
    convw = inp("convw", [128, cfg.DEPTH, 6, 5])
    dnsc = inp("dnsc", [128, cfg.DEPTH, 2, 4])
    normw = inp("normw", [128, cfg.DEPTH, 3])
    router = inp("router", [128, max(cfg.NMOE, 1), KC, E]) if cfg.NMOE else None
    ident_d = inp("ident", [128, 128])
    negmask_d = inp("negmask", [128, 8, 128])
    tri_d = inp("tri", [128, 2, 128])
    blkmask_d = inp("blkmask", [128, 7, 128])
    roperot_d = inp("roperot", [128, 128])
    ropecos_d = inp("ropecos", [128, L])
    ropesin_d = inp("ropesin", [128, L])
    dftc_d = inp("dftc", [128, 256], BF16)
    cosL_d = inp("cosL", [L, L // 4], BF16)
    sinL_d = inp("sinL", [L, L // 4], BF16)
    cosX_d = inp("cosX", [CTX, CTX], BF16)
    sinX_d = inp("sinX", [CTX, CTX], BF16)
    wmod_d = inp("wmod", [cfg.DEPTH, 6 * D, D // 4])
    wout_d = inp("wout", [cfg.DEPTH, D, 2048 // 4])
    fnw_d = inp("fnw", [cfg.DEPTH, 512, 128])
    ffg_d = inp("ffg", [cfg.NDENSE, cfg.DFF, D // 4])
    ffu_d = inp("ffu", [cfg.NDENSE, cfg.DFF, D // 4])
    ffd_d = inp("ffd", [cfg.NDENSE, D, cfg.DFF // 4])
    if cfg.NMOE:
        mog_d = inp("mog", [cfg.NMOE, E, cfg.DFF, D // 4])
        mou_d = inp("mou", [cfg.NMOE, E, cfg.DFF, D // 4])
        mod_d = inp("mod", [cfg.NMOE, E, D, cfg.DFF // 4])
    out_d = P.dram("out", [D, NL], F32, kind="ExternalOutput")
    dbg = {}

    def dbg_out(name, shape, dt=F32):
        if debug:
            dbg[name] = P.dram("dbg_" + name, shape, dt, kind="ExternalOutput")
            return dbg[name]
        return None

    CC_MAX = 1024 * 1024
    NQ = 4

    class WS:
        def __init__(self, src_tt, src_ap, R, C, name, cast=True):
            self.NB, self.C, self.Cp = R // 128, C, C // NQ
            self.bpc = max(1, CC_MAX // (128 * self.Cp * 2))
            self.src_tt, self.src_ap, self.cast = src_tt, src_ap, cast
            self.sh = P.dram(name + "_sh", [R, self.Cp], BF16)
            self.full = P.dram(name + "_full", [NQ * R, self.Cp], BF16)
            self.chunks = []
            for o0 in range(0, self.NB, self.bpc):
                o1 = min(self.NB, o0 + self.bpc)
                cs_, cf_ = TT(self.sh.h, name + "_s%d" % o0), TT(self.full.h, name + "_f%d" % o0)
                cs_.dsem = "WCAST" if cast else "WCOPY"
                cf_.dsem = "WAG"
                self.chunks.append((o0, o1, cs_, cf_))

        def emit_casts(self):
            for (o0, o1, cs_, cf_) in self.chunks:
                a, b = o0 * 128, o1 * 128
                P.dma("pool" if self.cast else "sp", self.sh.h[a:b, :], self.src_ap[a:b, :], reads=[self.src_tt], writes=[cs_])

        def emit_gathers(self):
            for (o0, o1, cs_, cf_) in self.chunks:
                a, b = o0 * 128, o1 * 128
                P.allgather(cs_, cf_, GRP, self.sh.h[a:b, :], self.full.h[NQ * a:NQ * b, :])
            for ch in self.chunks:
                ch[3].w = ("WAG", P.cnt["WAG"])

        def ttof(self, o):
            return self.chunks[o // self.bpc][3]

        def blk(self, o, q0=0, q1=NQ):
            o0, o1 = self.chunks[o // self.bpc][0:2]
            v = self.full.h[NQ * o0 * 128:NQ * o1 * 128, :].rearrange("(q o p) c -> p o q c", q=NQ, p=128)
            return v[:, o - o0, q0:q1, :]

    P._mksem("WCAST")
    P._mksem("WCOPY")
    P._mksem("WAG")
    W = {}
    win_bf = P.dram("win_bf", [cfg.DEPTH, 13 * 128, D], BF16)
    win_bf_l = [TT(win_bf.h, "win_bf%d" % l) for l in range(cfg.DEPTH)]
    for t_ in win_bf_l:
        t_.dsem = "WCAST"

    pending = {}

    def prep_layer(l):
        prep_casts(l)
        prep_gathers(l)

    def prep_casts(l):
        ws = []
        ws.append(("wmod", l, WS(wmod_d, wmod_d.h[l], 6 * D, D, "wmod%d" % l)))
        ws.append(("fnw", l, WS(fnw_d, fnw_d.h[l], 512, 512, "fnw%d" % l)))
        ws.append(("wout", l, WS(wout_d, wout_d.h[l], D, 2048, "wout%d" % l)))
        i = l // 2
        if l % 2 == 0:
            ws.append(("g", l, 0, WS(ffg_d, ffg_d.h[i], cfg.DFF, D, "ffg%d" % i)))
            ws.append(("u", l, 0, WS(ffu_d, ffu_d.h[i], cfg.DFF, D, "ffu%d" % i)))
            ws.append(("d", l, 0, WS(ffd_d, ffd_d.h[i], D, cfg.DFF, "ffd%d" % i)))
        else:
            for e in range(E):
                ws.append(("g", l, e, WS(mog_d, mog_d.h[i, e], cfg.DFF, D, "mog%d_%d" % (i, e))))
                ws.append(("u", l, e, WS(mou_d, mou_d.h[i, e], cfg.DFF, D, "mou%d_%d" % (i, e))))
                ws.append(("d", l, e, WS(mod_d, mod_d.h[i, e], D, cfg.DFF, "mod%d_%d" % (i, e))))
        P.dma("pool", win_bf.h[l], win.h[l], reads=[win], writes=[win_bf_l[l]])
        for it in ws:
            it[-1].emit_casts()
        tok = ("WCAST", P.cnt["WCAST"])
        win_bf_l[l].w = tok
        for it in ws:
            for ch in it[-1].chunks:
                ch[2].w = tok
        pending[l] = ws

    def prep_gathers(l):
        for it in pending.pop(l):
            it[-1].emit_gathers()
            W[it[:-1]] = it[-1]

    cosL = WS(cosL_d, cosL_d.h, L, L, "cosL", cast=False)
    sinL = WS(sinL_d, sinL_d.h, L, L, "sinL", cast=False)

    xres = P.dram("xres", [D, TOK], F32)
    h1_loc = P.dram("h1_loc", [KC, 128, TOK], BF16)
    h1_all = P.dram("h1_all", [KC, 4 * 128, TOK], BF16)
    pm = P.dram("pm", [13 * 128, S], F32)
    m_loc = P.dram("m_loc", [4, 4, 128, TOK], BF16)
    m_all = P.dram("m_all", [4, 4, 4 * 128, TOK], BF16)

    def mloc_write(i, s0, W_, src_tt, src_fn):
        if s0 < CTX:
            assert s0 == 0 and W_ == CTX
            for j in range(4):
                P.dma("sp", m_loc.h[i, j, :, NL:NL + NCX], src_fn(j * NCX, (j + 1) * NCX), reads=[src_tt], writes=[m_loc])
            return
        a0 = s0 - CTX
        a, end = a0, a0 + W_
        while a < end:
            j = a // NL
            b = min(end, (j + 1) * NL)
            P.dma("sp", m_loc.h[i, j, :, a - j * NL:b - j * NL], src_fn(a - a0, b - a0), reads=[src_tt], writes=[m_loc])
            a = b
    P.dma("sp", xres.h.ap(), xT.h.ap(), reads=[xT], writes=[xres])

    ident = P.sb("ident", [128, 128], F32)
    identb = P.sb("identb", [128, 128], BF16)
    ones_f = P.sb("ones_f", [128, 128], F32)
    ones_b = P.sb("ones_b", [128, 128], BF16)
    modsb = P.sb("modsb", [128, cfg.DEPTH, 6 * KC, 2], F32)
    bm_sb = P.sb("bm_sb", [128, cfg.DEPTH, 6 * KC], F32)
    lnp_sb = P.sb("lnp_sb", [128, cfg.DEPTH, 4, KC], F32)
    P.dma("sp", ident[:], ident_d.h.ap(), reads=[ident_d], writes=[ident])
    P.dma("sp", bm_sb[:], bmod.h.ap(), reads=[bmod], writes=[bm_sb])
    P.dma("sp", lnp_sb[:], lnp.h.ap(), reads=[lnp], writes=[lnp_sb])
    P.op("dve", lambda e: e.tensor_copy(identb[:], ident[:]), reads=[ident], writes=[identb])
    P.op("dve", lambda e: e.memset(ones_f[:], 1.0), writes=[ones_f])
    P.op("dve", lambda e: e.memset(ones_b[:], 1.0), writes=[ones_b])
    eps_sb = P.sb("eps_sb", [128, 1], F32)
    P.op("dve", lambda e: e.memset(eps_sb[:], EPS), writes=[eps_sb])
    pA, pB, pC, pS = [], [], [], []

    def std_psum():
        pA[:] = [P.ps("pA%d" % i, [128, 512]) for i in range(2)]
        pB[:] = [P.ps("pB%d" % i, [128, 512]) for i in range(2)]
        pC[:] = [P.ps("pC%d" % i, [128, 512]) for i in range(2)]
        pS[:] = [P.ps("pS%d" % i, [128, 512]) for i in range(2)]

    def fmview(t, c0, W_):
        return t.h.ap().rearrange("(k p) t -> p k t", p=128)[:, :, c0:c0 + W_]

    def ln_stats(xt, W_, sqb, mean, rstd, tmp):
        for kc in range(KC):
            P.op("pe", lambda e, kc=kc: e.matmul(pS[0][:, :W_], ones_f[:], xt[:, kc, :W_], start=(kc == 0), stop=(kc == KC - 1)),
                 reads=[ones_f, xt], writes=[pS[0]])
            sq = sqb[kc % 2]
            P.op("act", lambda e, kc=kc, sq=sq: e.activation(sq[:, :W_], xt[:, kc, :W_], AF.Square), reads=[xt], writes=[sq])
            P.op("pe", lambda e, kc=kc, sq=sq: e.matmul(pS[1][:, :W_], ones_f[:], sq[:, :W_], start=(kc == 0), stop=(kc == KC - 1)),
                 reads=[ones_f, sq], writes=[pS[1]])
        P.op("dve", lambda e: e.tensor_scalar(mean[:, :W_], pS[0][:, :W_], 1.0 / D, None, op0=ALU.mult), reads=[pS[0]], writes=[mean])
        P.op("dve", lambda e: e.tensor_tensor(tmp[:, :W_], mean[:, :W_], mean[:, :W_], ALU.mult), reads=[mean], writes=[tmp])
        P.op("dve", lambda e: e.scalar_tensor_tensor(tmp[:, :W_], pS[1][:, :W_], 1.0 / D, tmp[:, :W_], op0=ALU.mult, op1=ALU.subtract),
             reads=[pS[1], tmp], writes=[tmp])
        P.op("act", lambda e: e.activation(rstd[:, :W_], tmp[:, :W_], AF.Sqrt, bias=eps_sb[:, 0:1]), reads=[tmp, eps_sb], writes=[rstd])
        P.op("dve", lambda e: e.reciprocal(rstd[:, :W_], rstd[:, :W_]), reads=[rstd], writes=[rstd])

    def ln_apply(xt, W_, mean, rstd, tA, tB, scale_fn, bias_fn, out_fn, out_t, extra_reads=()):
        for kc in range(KC):
            t1, t2 = tA[kc % 2], tB[kc % 2]
            P.op("dve", lambda e, kc=kc, t1=t1: e.tensor_tensor(t1[:, :W_], xt[:, kc, :W_], mean[:, :W_], ALU.subtract),
                 reads=[xt, mean], writes=[t1])
            P.op("pool", lambda e, t1=t1, t2=t2: e.tensor_tensor(t2[:, :W_], t1[:, :W_], rstd[:, :W_], ALU.mult),
                 reads=[t1, rstd], writes=[t2])
            P.op("act", lambda e, kc=kc, t2=t2: e.activation(out_fn(kc), t2[:, :W_], AF.Identity, bias=bias_fn(kc), scale=scale_fn(kc)),
                 reads=[t2] + list(extra_reads), writes=[out_t])

    cv = P.sb("cv", [128, KC, 2], F32)
    cvb = P.sb("cvb", [128, KC, 2], BF16)
    P.dma("sp", cv[:], cvec.h.ap(), reads=[cvec], writes=[cv])
    P.op("act", lambda e: e.activation(cvb[:], cv[:], AF.Silu), reads=[cv], writes=[cvb])

    def P0(l):
        with P.scope():
            std_psum()
            wb = [P.sb("wmodb%d" % i, [128, KC * 128], BF16) for i in range(3)]
            n = 0
            wf = W["wmod", l]
            for o in range(6 * KC):
                w = wb[n % 3]
                n += 1
                P.dma("sp", w.h[:].rearrange("p (q c) -> p q c", q=NQ), wf.blk(o), reads=[wf.ttof(o)], writes=[w])
                ps = pA[o % 2]
                for kc in range(KC):
                    P.op("pe", lambda e, w=w, kc=kc, ps=ps: e.matmul(ps[:, 0:2], w[:, kc * 128:(kc + 1) * 128], cvb[:, kc, :],
                                                                     start=(kc == 0), stop=(kc == KC - 1)),
                         reads=[w, cvb], writes=[ps])
                isscale = (o // KC) in (1, 4)
                P.op("dve", lambda e, l=l, o=o, ps=ps, a=(1.0 if isscale else 0.0): e.tensor_scalar(
                    modsb[:, l, o, :], ps[:, 0:2], bm_sb[:, l, o:o + 1], a, op0=ALU.add, op1=ALU.add),
                    reads=[ps, bm_sb], writes=[modsb])

    def P1(l):
        with P.scope():
            std_psum()
            xts = [P.sb("xt%d" % i, [128, KC, TW], F32) for i in range(2)]
            hbs = [P.sb("hb%d" % i, [128, KC, TW], BF16) for i in range(2)]
            sqb = [P.sb("sq%d" % i, [128, TW], F32) for i in range(2)]
            tA = [P.sb("tA%d" % i, [128, TW], F32) for i in range(2)]
            tB = [P.sb("tB%d" % i, [128, TW], F32) for i in range(2)]
            mean = P.sb("mean", [128, TW], F32)
            rstd = P.sb("rstd", [128, TW], F32)
            tmp = P.sb("tmp", [128, TW], F32)
            for ti, (c0, W_, isctx) in enumerate(cfg.ttiles):
                col = 1 if isctx else 0
                xt, hb = xts[ti % 2], hbs[ti % 2]
                P.dma("sp", xt[:, :, :W_], fmview(xres, c0, W_), reads=[xres], writes=[xt])
                ln_stats(xt, W_, sqb, mean, rstd, tmp)
                ln_apply(xt, W_, mean, rstd, tA, tB,
                         lambda kc: modsb[:, l, KC + kc, col:col + 1], lambda kc: modsb[:, l, kc, col:col + 1],
                         lambda kc, hb=hb: hb[:, kc, :W_], hb, extra_reads=[modsb])
                P.dma("sp", h1_loc.h.ap().rearrange("k p t -> p k t")[:, :, c0:c0 + W_], hb[:, :, :W_], reads=[hb], writes=[h1_loc])

    def seq_tiles():
        tiles = [(0, CTX, [(r, NL, NCX, r * NCX) for r in range(4)])]
        for r in range(4):
            for i in range(NL // TW):
                tiles.append((CTX + r * NL + i * TW, TW, [(r, i * TW, TW, 0)]))
        return tiles

    def P2(l):
        with P.scope():
            std_psum()
            wown = P.sb("wown", [128, 13, KC * 128], BF16)
            P.dma("sp", wown[:], win_bf.h[l].rearrange("(o p) k -> p o k", p=128), reads=[win_bf_l[l]], writes=[wown])
            h1ts = [P.sb("h1t%d" % i, [128, KC, TW], BF16) for i in range(2)]
            pos = [P.sb("po%d" % i, [128, TW], F32) for i in range(3)]
            n = 0
            for ti, (s0, W_, pieces) in enumerate(seq_tiles()):
                ht = h1ts[ti % 2]
                for (r, c0, w, off) in pieces:
                    src = h1_all.h[:, r * 128:(r + 1) * 128, c0:c0 + w].rearrange("k p t -> p k t")
                    P.dma("sp", ht[:, :, off:off + w], src, reads=[h1_all], writes=[ht])
                for o in range(13):
                    ps = pA[o % 2]
                    for kc in range(KC):
                        P.op("pe", lambda e, o=o, kc=kc, ps=ps, ht=ht: e.matmul(
                            ps[:, :W_], wown[:, o, kc * 128:(kc + 1) * 128], ht[:, kc, :W_], start=(kc == 0), stop=(kc == KC - 1)),
                            reads=[wown, ht], writes=[ps])
                    po = pos[n % 3]
                    n += 1
                    P.op("act", lambda e, ps=ps, po=po: e.activation(po[:, :W_], ps[:, :W_], AF.Identity), reads=[ps], writes=[po])
                    P.dma("sp", pm.h[o * 128:(o + 1) * 128, s0:s0 + W_], po[:, :W_], reads=[po], writes=[pm])

    sel_d = inp("sel", [8, 8, 128])
    sel_sb = P.sb("sel_sb", [8, 8, 128], F32)
    P.dma("sp", sel_sb[:], sel_d.h.ap(), reads=[sel_d], writes=[sel_sb])

    jreg = {}

    def P4(l, last):
        moe = (l % 2 == 1)
        im = l // 2
        ne = E if moe else 1
        with P.scope():
            std_psum()
            xt = P.sb("xt", [128, KC, TW], F32)
            mt = P.sb("mt", [128, max(16, KC), TW], BF16)
            flt = P.sb("flt", [128, 4, TW], BF16)
            h2 = mt
            act = P.sb("act", [128, FC, TW], BF16)
            fnw_sb = P.sb("fnw_sb", [128, 4, 512], BF16)
            wob = [P.sb("wob%d" % i, [128, 16 * 128], BF16) for i in range(2)]
            wgb = [P.sb("wgb%d" % i, [128, KC * 128], BF16) for i in range(2)]
            wub = [P.sb("wub%d" % i, [128, KC * 128], BF16) for i in range(2)]
            FH = FC // 2
            wdb = [P.sb("wdb%d" % i, [128, FH * 128], BF16) for i in range(3)]
            sqb = [P.sb("sq%d" % i, [128, TW], F32) for i in range(2)]
            tA = [P.sb("tA%d" % i, [128, TW], F32) for i in range(2)]
            tB = [P.sb("tB%d" % i, [128, TW], F32) for i in range(2)]
            sgt = [P.sb("sg%d" % i, [128, TW], F32) for i in range(2)]
            mean = P.sb("mean", [128, TW], F32)
            rstd = P.sb("rstd", [128, TW], F32)
            tmp = P.sb("tmp", [128, TW], F32)
            if moe:
                rt_sb = P.sb("rt_sb", [128, KC, E], F32)
                P.dma("sp", rt_sb[:], router.h[:, im], reads=[router], writes=[rt_sb])
                h32 = [P.sb("h32_%d" % i, [128, TW], F32) for i in range(2)]
                lgT = P.sb("lgT", [8, TW], F32)
                NB4 = (TW + 127) // 128
                lg = P.sb("lg", [128, NB4, E], F32)
                lg2 = P.sb("lg2", [128, NB4, E], F32)
                eq1 = P.sb("eq1", [128, NB4, E], F32)
                eq2 = P.sb("eq2", [128, NB4, E], F32)
                comb = P.sb("comb", [128, NB4, E], F32)
                m1 = P.sb("m1", [128, NB4], F32)
                m2 = P.sb("m2", [128, NB4], F32)
                dm = P.sb("dm", [128, NB4], F32)
                g1 = P.sb("g1", [128, NB4], F32)
                g2 = P.sb("g2", [128, NB4], F32)
                combT = P.sb("combT", [8, TW], F32)
                cb = [P.sb("cb%d" % e_, [128, TW], F32) for e_ in range(E)]
            for o_ in range(4):
                P.dma("sp", fnw_sb.h[:, o_, :].rearrange("p (q c) -> p q c", q=NQ), W["fnw", l].blk(o_), reads=[W["fnw", l].ttof(o_)], writes=[fnw_sb])
            nwo = nwg = nwd = 0
            for ti, (c0, W_, isctx) in enumerate(cfg.ttiles):
                col = 1 if isctx else 0
                if mt.dsem is None:
                    mt.dsem = P._getdsem()
                semh = P.sem[mt.dsem]
                for i_ in range(4):
                    P._deps("sp", [m_all], [mt])
                    P.cnt[mt.dsem] += 16

                    def ldm(e, i_=i_, c0=c0, W_=W_, semh=semh):
                        if "j" not in jreg:
                            jreg["j"] = e.partition_id() % 4
                        j = jreg["j"]
                        src = m_all.h[:, i_].rearrange("j (r p) t -> p j r t", p=128)[:, bass.ds(j, 1), :, c0:c0 + W_]
                        return e.dma_start(out=mt.h[:, 4 * i_:4 * i_ + 4, :W_], in_=src).then_inc(semh, 16)
                    P.ops["sp"].append(ldm)
                    P._mark((mt.dsem, P.cnt[mt.dsem]), [m_all], [mt])
                P.dma("sp", xt[:, :, :W_], fmview(xres, c0, W_), reads=[xres], writes=[xt])
                if P4_STOP == "mt":
                    return
                for o in range(4):
                    ps = pA[o % 2]
                    for r in range(4):
                        P.op("pe", lambda e, o=o, r=r, ps=ps: e.matmul(ps[:, :W_], fnw_sb[:, o, r * 128:(r + 1) * 128], mt[:, 12 + r, :W_],
                                                                      start=(r == 0), stop=(r == 3)), reads=[fnw_sb, mt], writes=[ps])
                    P.op("act", lambda e, o=o, ps=ps: e.activation(flt[:, o, :W_], ps[:, :W_], AF.Identity), reads=[ps], writes=[flt])
                if P4_STOP == "fnet":
                    return
                for d in range(KC):
                    wo = wob[nwo % 2]
                    nwo += 1
                    P.dma("sp", wo.h[:].rearrange("p (q c) -> p q c", q=NQ), W["wout", l].blk(d), reads=[W["wout", l].ttof(d)], writes=[wo])
                    ps = pC[d % 2]
                    for kc in range(16):
                        rhs_t, rhs = (flt, flt[:, kc - 12, :W_]) if kc >= 12 else (mt, mt[:, kc, :W_])
                        P.op("pe", lambda e, wo=wo, kc=kc, ps=ps, rhs=rhs: e.matmul(ps[:, :W_], wo[:, kc * 128:(kc + 1) * 128], rhs,
                                                                                 start=(kc == 0), stop=(kc == 15)),
                             reads=[wo, rhs_t], writes=[ps])
                    P.op("act", lambda e, d=d: e.activation(xt[:, d, :W_], xt[:, d, :W_], AF.Identity, scale=cfg.alpha), reads=[xt], writes=[xt])
                    P.op("dve", lambda e, d=d, ps=ps: e.scalar_tensor_tensor(xt[:, d, :W_], ps[:, :W_], modsb[:, l, 2 * KC + d, col:col + 1],
                                                                            xt[:, d, :W_], op0=ALU.mult, op1=ALU.add),
                         reads=[ps, xt, modsb], writes=[xt])
                if P4_STOP == "outproj":
                    return
                ln_stats(xt, W_, sqb, mean, rstd, tmp)
                ln_apply(xt, W_, mean, rstd, tA, tB, lambda kc: lnp_sb[:, l, 0, kc:kc + 1], lambda kc: lnp_sb[:, l, 1, kc:kc + 1],
                         lambda kc: xt[:, kc, :W_], xt, extra_reads=[lnp_sb])
                ln_stats(xt, W_, sqb, mean, rstd, tmp)
                if not moe:
                    ln_apply(xt, W_, mean, rstd, tA, tB, lambda kc: modsb[:, l, 4 * KC + kc, col:col + 1],
                             lambda kc: modsb[:, l, 3 * KC + kc, col:col + 1], lambda kc: h2[:, kc, :W_], h2, extra_reads=[modsb])
                else:
                    for kc in range(KC):
                        t1, t2, hh = tA[kc % 2], tB[kc % 2], h32[kc % 2]
                        P.op("dve", lambda e, kc=kc, t1=t1: e.tensor_tensor(t1[:, :W_], xt[:, kc, :W_], mean[:, :W_], ALU.subtract),
                             reads=[xt, mean], writes=[t1])
                        P.op("pool", lambda e, t1=t1, t2=t2: e.tensor_tensor(t2[:, :W_], t1[:, :W_], rstd[:, :W_], ALU.mult),
                             reads=[t1, rstd], writes=[t2])
                        P.op("act", lambda e, kc=kc, t2=t2, hh=hh: e.activation(hh[:, :W_], t2[:, :W_], AF.Identity,
                                                                             bias=modsb[:, l, 3 * KC + kc, col:col + 1],
                                                                             scale=modsb[:, l, 4 * KC + kc, col:col + 1]),
                             reads=[t2, modsb], writes=[hh])
                        P.op("pool", lambda e, kc=kc, hh=hh: e.tensor_copy(h2[:, kc, :W_], hh[:, :W_]), reads=[hh], writes=[h2])
                        P.op("pe", lambda e, kc=kc, hh=hh: e.matmul(pS[0][0:E, :W_], rt_sb[:, kc, :], hh[:, :W_], start=(kc == 0), stop=(kc == KC - 1)),
                             reads=[rt_sb, hh], writes=[pS[0]])
                    P.op("dve", lambda e: e.tensor_copy(lgT[:, :W_], pS[0][0:E, :W_]), reads=[pS[0]], writes=[lgT])
                    nb = (W_ + 127) // 128
                    for bi in range(nb):
                        wbk = min(128, W_ - bi * 128)
                        P.op("pe", lambda e, bi=bi, wbk=wbk: e.transpose(pS[1][:wbk, bi * 8:bi * 8 + E], lgT[0:E, bi * 128:bi * 128 + wbk], ident[0:E, 0:E]),
                             reads=[lgT, ident], writes=[pS[1]])
                    P.op("dve", lambda e: e.memset(lg[:], -1.0e30), writes=[lg])
                    for bi in range(nb):
                        wbk = min(128, W_ - bi * 128)
                        P.op("dve", lambda e, bi=bi, wbk=wbk: e.tensor_copy(lg[:wbk, bi, :], pS[1][:wbk, bi * 8:bi * 8 + E]), reads=[pS[1]], writes=[lg])
                    P.op("dve", lambda e: e.tensor_reduce(m1[:, :nb], lg[:, :nb, :], mybir.AxisListType.X, ALU.max), reads=[lg], writes=[m1])
                    for bi in range(nb):
                        P.op("dve", lambda e, bi=bi: e.tensor_scalar(eq1[:, bi, :], lg[:, bi, :], m1[:, bi:bi + 1], None, op0=ALU.is_equal),
                             reads=[lg, m1], writes=[eq1])
                    P.op("dve", lambda e: e.scalar_tensor_tensor(lg2[:, :nb, :], eq1[:, :nb, :], -1.0e30, lg[:, :nb, :], op0=ALU.mult, op1=ALU.add),
                         reads=[eq1, lg], writes=[lg2])
                    P.op("dve", lambda e: e.tensor_reduce(m2[:, :nb], lg2[:, :nb, :], mybir.AxisListType.X, ALU.max), reads=[lg2], writes=[m2])
                    for bi in range(nb):
                        P.op("dve", lambda e, bi=bi: e.tensor_scalar(eq2[:, bi, :], lg2[:, bi, :], m2[:, bi:bi + 1], None, op0=ALU.is_equal),
                             reads=[lg2, m2], writes=[eq2])
                    P.op("dve", lambda e: e.tensor_tensor(dm[:, :nb], m1[:, :nb], m2[:, :nb], ALU.subtract), reads=[m1, m2], writes=[dm])
                    P.op("act", lambda e: e.activation(g1[:, :nb], dm[:, :nb], AF.Sigmoid), reads=[dm], writes=[g1])
                    P.op("act", lambda e: e.activation(g2[:, :nb], dm[:, :nb], AF.Sigmoid, scale=-1.0), reads=[dm], writes=[g2])
                    for bi in range(nb):
                        P.op("dve", lambda e, bi=bi: e.tensor_scalar(comb[:, bi, :], eq1[:, bi, :], g1[:, bi:bi + 1], None, op0=ALU.mult),
                             reads=[eq1, g1], writes=[comb])
                        P.op("dve", lambda e, bi=bi: e.scalar_tensor_tensor(comb[:, bi, :], eq2[:, bi, :], g2[:, bi:bi + 1], comb[:, bi, :],
                                                                          op0=ALU.mult, op1=ALU.add), reads=[eq2, g2, comb], writes=[comb])
                    for bi in range(nb):
                        wbk = min(128, W_ - bi * 128)
                        P.op("pe", lambda e, bi=bi, wbk=wbk: e.transpose(pS[0][0:E, bi * 128:bi * 128 + wbk], comb[:wbk, bi, :], ident[:wbk, :wbk]),
                             reads=[comb, ident], writes=[pS[0]])
                    P.op("dve", lambda e: e.tensor_copy(combT[:, :W_], pS[0][0:E, :W_]), reads=[pS[0]], writes=[combT])
                    for e_ in range(E):
                        ps = pS[e_ % 2]
                        P.op("pe", lambda e, e_=e_, ps=ps: e.matmul(ps[:, :W_], sel_sb[:, e_, :], combT[:, :W_], start=True, stop=True),
                             reads=[sel_sb, combT], writes=[ps])
                        P.op("act", lambda e, e_=e_, ps=ps: e.activation(cb[e_][:, :W_], ps[:, :W_], AF.Identity), reads=[ps], writes=[cb[e_]])
                if P4_STOP == "h2":
                    return
                for kc in range(KC):
                    P.op("act", lambda e, kc=kc: e.activation(xt[:, kc, :W_], xt[:, kc, :W_], AF.Identity, scale=cfg.alpha), reads=[xt], writes=[xt])
                for e_ in range(ne):
                    wg_f, wu_f, wd_f = W["g", l, e_], W["u", l, e_], W["d", l, e_]
                    for f in range(FC):
                        wg, wu = wgb[nwg % 2], wub[nwg % 2]
                        nwg += 1
                        P.dma("sp", wg.h[:].rearrange("p (q c) -> p q c", q=NQ), wg_f.blk(f), reads=[wg_f.ttof(f)], writes=[wg])
                        P.dma("sp", wu.h[:].rearrange("p (q c) -> p q c", q=NQ), wu_f.blk(f), reads=[wu_f.ttof(f)], writes=[wu])
                        pg, pu = pA[f % 2], pB[f % 2]
                        for kc in range(KC):
                            P.op("pe", lambda e, wg=wg, kc=kc, pg=pg: e.matmul(pg[:, :W_], wg[:, kc * 128:(kc + 1) * 128], h2[:, kc, :W_],
                                                                             start=(kc == 0), stop=(kc == KC - 1)), reads=[wg, h2], writes=[pg])
                        for kc in range(KC):
                            P.op("pe", lambda e, wu=wu, kc=kc, pu=pu: e.matmul(pu[:, :W_], wu[:, kc * 128:(kc + 1) * 128], h2[:, kc, :W_],
                                                                             start=(kc == 0), stop=(kc == KC - 1)), reads=[wu, h2], writes=[pu])
                        sg = sgt[f % 2]
                        P.op("act", lambda e, pg=pg, sg=sg: e.activation(sg[:, :W_], pg[:, :W_], AF.Silu), reads=[pg], writes=[sg])
                        if moe:
                            P.op("pool", lambda e, sg=sg, e_=e_: e.tensor_tensor(sg[:, :W_], sg[:, :W_], cb[e_][:, :W_], ALU.mult),
                                 reads=[sg, cb[e_]], writes=[sg])
                        P.op("dve", lambda e, f=f, sg=sg, pu=pu: e.tensor_tensor(act[:, f, :W_], sg[:, :W_], pu[:, :W_], ALU.mult),
                             reads=[sg, pu], writes=[act])
                    for d in range(KC):
                        ps = pC[d % 2]
                        for hf in range(2):
                            wd = wdb[nwd % 3]
                            nwd += 1
                            P.dma("sp", wd.h[:].rearrange("p (q c) -> p q c", q=2), wd_f.blk(d, 2 * hf, 2 * hf + 2), reads=[wd_f.ttof(d)], writes=[wd])
                            for f2 in range(FH):
                                f = hf * FH + f2
                                P.op("pe", lambda e, wd=wd, f=f, f2=f2, ps=ps: e.matmul(ps[:, :W_], wd[:, f2 * 128:(f2 + 1) * 128], act[:, f, :W_],
                                                                                     start=(f == 0), stop=(f == FC - 1)), reads=[wd, act], writes=[ps])
                        P.op("dve", lambda e, d=d, ps=ps: e.scalar_tensor_tensor(xt[:, d, :W_], ps[:, :W_], modsb[:, l, 5 * KC + d, col:col + 1],
                                                                                xt[:, d, :W_], op0=ALU.mult, op1=ALU.add),
                             reads=[ps, xt, modsb], writes=[xt])
                if P4_STOP == "ffn":
                    return
                ln_stats(xt, W_, sqb, mean, rstd, tmp)
                ln_apply(xt, W_, mean, rstd, tA, tB, lambda kc: lnp_sb[:, l, 2, kc:kc + 1], lambda kc: lnp_sb[:, l, 3, kc:kc + 1],
                         lambda kc: xt[:, kc, :W_], xt, extra_reads=[lnp_sb])
                P.dma("sp", fmview(xres, c0, W_), xt[:, :, :W_], reads=[xt], writes=[xres])
                if last and not isctx:
                    P.dma("sp", fmview(out_d, c0, W_), xt[:, :, :W_], reads=[xt], writes=[out_d])

    nw_sb = P.sb("nw_sb", [128, cfg.DEPTH, 3], F32)
    P.dma("sp", nw_sb[:], normw.h.ap(), reads=[normw], writes=[nw_sb])
    NBLK = S // 128
    CB = CTX // 128

    def P3c(l):
        with P.scope():
            std_psum()
            dftc_sb = P.sb("dftc_sb", [128, 256], BF16)
            P.dma("sp", dftc_sb[:], dftc_d.h.ap(), reads=[dftc_d], writes=[dftc_sb])
            ut = [P.sb("ut%d" % i, [128, 512], F32) for i in range(2)]
            utb = [P.sb("utb%d" % i, [128, 512], BF16) for i in range(2)]
            AB = P.sb("AB", [128, max(L // 128, 1), 256], BF16)
            SG = 16
            cst = [P.sb("cst%d" % i, [128, SG, 512], BF16) for i in range(2)]
            sst = [P.sb("sst%d" % i, [128, SG, 512], BF16) for i in range(2)]
            fo = [P.sb("fo%d" % i, [128, 512], BF16) for i in range(2)]
            nt = 0
            for (seq0, n, ctab, stab) in ((0, CTX, cosX_d, sinX_d), (CTX, L, cosL, sinL)):
                nb = n // 128
                Wt = min(512, n)
                for i in range(n // Wt):
                    u_, ub_ = ut[i % 2], utb[i % 2]
                    P.dma("sp", u_[:, :Wt], pm.h[11 * 128:12 * 128, seq0 + i * Wt: seq0 + (i + 1) * Wt], reads=[pm], writes=[u_])
                    P.op("dve", lambda e, u_=u_, ub_=ub_: e.tensor_copy(ub_[:, :Wt], u_[:, :Wt]), reads=[u_], writes=[ub_])
                    for k in range(Wt // 128):
                        sb_ = i * (Wt // 128) + k
                        ps = pA[sb_ % 2]
                        P.op("pe", lambda e, ub_=ub_, k=k, ps=ps: e.matmul(ps[:, 0:256], ub_[:, k * 128:(k + 1) * 128], dftc_sb[:], start=True, stop=True),
                             reads=[ub_, dftc_sb], writes=[ps])
                        P.op("act", lambda e, sb_=sb_, ps=ps: e.activation(AB[:, sb_, :], ps[:, 0:256], AF.Identity), reads=[ps], writes=[AB])
                for ti in range(n // Wt):
                    ps = pC[ti % 2]
                    ng = (nb + SG - 1) // SG
                    for g in range(ng):
                        g0 = g * SG
                        gn = min(SG, nb - g0)
                        ct, st_ = cst[nt % 2], sst[nt % 2]
                        nt += 1
                        if isinstance(ctab, WS):
                            for tab, tl in ((ctab, ct), (stab, st_)):
                                for k in range(gn):
                                    cp = tab.Cp
                                    if Wt >= cp:
                                        P.dma("sp", tl.h[:, k, :Wt].rearrange("p (q c) -> p q c", c=cp),
                                              tab.blk(g0 + k, (ti * Wt) // cp, ((ti + 1) * Wt) // cp), reads=[tab.ttof(g0 + k)], writes=[tl])
                                    else:
                                        q_, off_ = (ti * Wt) // cp, (ti * Wt) % cp
                                        P.dma("sp", tl.h[:, k, :Wt], tab.blk(g0 + k, q_, q_ + 1)[:, 0, off_:off_ + Wt], reads=[tab.ttof(g0 + k)], writes=[tl])
                        else:
                            P.dma("sp", ct[:, :gn, :Wt], ctab.h.ap().rearrange("(s p) t -> p s t", p=128)[:, g0:g0 + gn, ti * Wt:(ti + 1) * Wt],
                                  reads=[ctab], writes=[ct])
                            P.dma("sp", st_[:, :gn, :Wt], stab.h.ap().rearrange("(s p) t -> p s t", p=128)[:, g0:g0 + gn, ti * Wt:(ti + 1) * Wt],
                                  reads=[stab], writes=[st_])
                        for k in range(gn):
                            sb_ = g0 + k
                            P.op("pe", lambda e, sb_=sb_, k=k, ct=ct, ps=ps: e.matmul(ps[:, :Wt], AB[:, sb_, 0:128], ct[:, k, :Wt],
                                                                                   start=(sb_ == 0), stop=False), reads=[AB, ct], writes=[ps])
                            P.op("pe", lambda e, sb_=sb_, k=k, st_=st_, ps=ps: e.matmul(ps[:, :Wt], AB[:, sb_, 128:256], st_[:, k, :Wt],
                                                                                     start=False, stop=(sb_ == nb - 1)), reads=[AB, st_], writes=[ps])
                    f_ = fo[ti % 2]
                    P.op("act", lambda e, f_=f_, ps=ps: e.activation(f_[:, :Wt], ps[:, :Wt], AF.Identity), reads=[ps], writes=[f_])
                    mloc_write(3, seq0 + ti * Wt, Wt, f_, lambda a, b, f_=f_: f_[:, a:b])

    def P3b(l):
        with P.scope():
            std_psum()
            rot_sb = P.sb("rot_sb", [128, 128], F32)
            P.dma("sp", rot_sb[:], roperot_d.h.ap(), reads=[roperot_d], writes=[rot_sb])
            qT = P.sb("qT", [128, S], BF16)
            kT = P.sb("kT", [128, S], BF16)
            vtok = P.sb("vtok", [128, NBLK, 128], BF16)
            xin = [P.sb("xin%d" % i, [128, 512], F32) for i in range(2)]
            sq = P.sb("sqa", [128, 512], F32)
            rs = P.sb("rsa", [128, 512], F32)
            xn = P.sb("xna", [128, 512], F32)
            o1 = P.sb("o1a", [128, 512], F32)
            o2 = P.sb("o2a", [128, 512], F32)
            cs = [P.sb("cs%d" % i, [128, 512], F32) for i in range(2)]
            sn = [P.sb("sn%d" % i, [128, 512], F32) for i in range(2)]
            eb = [P.sb("eb%d" % i, [128, 512], BF16) for i in range(3)]
            rden = P.sb("rden", [128, 512], F32)
            ob = [P.sb("oba%d" % i, [128, 512], BF16) for i in range(2)]
            tiles = [(0, CTX, True)] + [(CTX + i * 512, min(512, L), False) for i in range(max(L // 512, 1))]
            if L < 512:
                tiles = [(0, CTX, True), (CTX, L, False)]
            n = 0
            for (s0, W_, isctx) in tiles:
                for which, dst, ncol in ((8, qT, 1), (9, kT, 2)):
                    x_ = xin[n % 2]
                    P.dma("sp", x_[:, :W_], pm.h[which * 128:(which + 1) * 128, s0:s0 + W_], reads=[pm], writes=[x_])
                    P.op("act", lambda e, x_=x_: e.activation(sq[:, :W_], x_[:, :W_], AF.Square), reads=[x_], writes=[sq])
                    P.op("pe", lambda e: e.matmul(pS[0][:, :W_], ones_f[:], sq[:, :W_], start=True, stop=True), reads=[ones_f, sq], writes=[pS[0]])
                    P.op("act", lambda e: e.activation(rs[:, :W_], pS[0][:, :W_], AF.Sqrt, bias=eps_sb[:, 0:1], scale=1.0 / 128), reads=[pS[0], eps_sb], writes=[rs])
                    P.op("dve", lambda e: e.reciprocal(rs[:, :W_], rs[:, :W_]), reads=[rs], writes=[rs])
                    if isctx:
                        P.op("dve", lambda e, x_=x_, dst=dst, ncol=ncol: e.scalar_tensor_tensor(dst[:, s0:s0 + W_], x_[:, :W_], nw_sb[:, l, ncol:ncol + 1], rs[:, :W_],
                                                                                               op0=ALU.mult, op1=ALU.mult), reads=[x_, nw_sb, rs], writes=[dst])
                    else:
                        c_, s_ = cs[n % 2], sn[n % 2]
                        P.dma("sp", c_[:, :W_], ropecos_d.h[:, s0 - CTX:s0 - CTX + W_], reads=[ropecos_d], writes=[c_])
                        P.dma("sp", s_[:, :W_], ropesin_d.h[:, s0 - CTX:s0 - CTX + W_], reads=[ropesin_d], writes=[s_])
                        P.op("dve", lambda e, x_=x_, ncol=ncol: e.scalar_tensor_tensor(xn[:, :W_], x_[:, :W_], nw_sb[:, l, ncol:ncol + 1], rs[:, :W_],
                                                                                     op0=ALU.mult, op1=ALU.mult), reads=[x_, nw_sb, rs], writes=[xn])
                        P.op("pe", lambda e: e.matmul(pS[1][:, :W_], rot_sb[:], xn[:, :W_], start=True, stop=True), reads=[rot_sb, xn], writes=[pS[1]])
                        P.op("pool", lambda e, c_=c_: e.tensor_tensor(o1[:, :W_], xn[:, :W_], c_[:, :W_], ALU.mult), reads=[xn, c_], writes=[o1])
                        P.op("dve", lambda e, s_=s_: e.tensor_tensor(o2[:, :W_], pS[1][:, :W_], s_[:, :W_], ALU.mult), reads=[pS[1], s_], writes=[o2])
                        P.op("dve", lambda e, dst=dst: e.tensor_tensor(dst[:, s0:s0 + W_], o1[:, :W_], o2[:, :W_], ALU.add), reads=[o1, o2], writes=[dst])
                    n += 1
                x_ = xin[n % 2]
                n += 1
                P.dma("sp", x_[:, :W_], pm.h[10 * 128:11 * 128, s0:s0 + W_], reads=[pm], writes=[x_])
                for k in range(W_ // 128):
                    blk_ = s0 // 128 + k
                    ps = pB[k % 2]
                    P.op("pe", lambda e, x_=x_, k=k, ps=ps: e.transpose(ps[:, 0:128], x_[:, k * 128:(k + 1) * 128], ident[:]), reads=[x_, ident], writes=[ps])
                    P.op("act", lambda e, blk_=blk_, ps=ps: e.activation(vtok[:, blk_, :], ps[:, 0:128], AF.Identity), reads=[ps], writes=[vtok])
            scale = 128 ** -0.5
            ne = 0
            for qi, (s0, W_, isctx) in enumerate(tiles):
                kblocks = list(range(CB)) if isctx else list(range(NBLK))
                for ki, kb in enumerate(kblocks):
                    ps = pA[ki % 2]
                    P.op("pe", lambda e, kb=kb, ps=ps: e.matmul(ps[:, :W_], kT[:, kb * 128:(kb + 1) * 128], qT[:, s0:s0 + W_], start=True, stop=True),
                         reads=[kT, qT], writes=[ps])
                    e_ = eb[ne % 3]
                    ne += 1
                    P.op("act", lambda e, e_=e_, ps=ps: e.activation(e_[:, :W_], ps[:, :W_], AF.Exp, scale=scale), reads=[ps], writes=[e_])
                    P.op("pe", lambda e, kb=kb, e_=e_, ki=ki: e.matmul(pC[0][:, :W_], vtok[:, kb, :], e_[:, :W_], start=(ki == 0), stop=(ki == len(kblocks) - 1)),
                         reads=[vtok, e_], writes=[pC[0]])
                    P.op("pe", lambda e, e_=e_, ki=ki: e.matmul(pC[1][:, :W_], ones_b[:], e_[:, :W_], start=(ki == 0), stop=(ki == len(kblocks) - 1)),
                         reads=[ones_b, e_], writes=[pC[1]])
                P.op("dve", lambda e: e.reciprocal(rden[:, :W_], pC[1][:, :W_]), reads=[pC[1]], writes=[rden])
                o_ = ob[qi % 2]
                P.op("dve", lambda e, o_=o_: e.tensor_tensor(o_[:, :W_], pC[0][:, :W_], rden[:, :W_], ALU.mult), reads=[pC[0], rden], writes=[o_])
                mloc_write(2, s0, W_, o_, lambda a, b, o_=o_: o_[:, a:b])

    odram = [[P.dram("odn_%d_%d" % (d_, h_), [128, S], F32) for h_ in range(2)] for d_ in range(2)]

    def P3a(l):
        with P.scope():
            banks = [P.ps("dnbank%d" % i, [128, 512]) for i in range(8)]
            pst = banks[0:4]
            pbig = banks[4:6]
            negm = P.sb("negm", [128, 8, 128], F32)
            tri = P.sb("tri", [128, 2, 128], F32)
            cw = P.sb("cw", [128, 6, 5], F32)
            dsc = P.sb("dsc", [128, 2, 4], F32)
            negA = P.sb("negA", [128, 4], F32)
            P.dma("sp", negm[:], negmask_d.h.ap(), reads=[negmask_d], writes=[negm])
            P.dma("sp", tri[:], tri_d.h.ap(), reads=[tri_d], writes=[tri])
            bmask = P.sb("bmask", [128, 7, 128], F32)
            P.dma("sp", bmask[:], blkmask_d.h.ap(), reads=[blkmask_d], writes=[bmask])
            P.dma("sp", cw[:], convw.h[:, l], reads=[convw], writes=[cw])
            P.dma("sp", dsc[:], dnsc.h[:, l], reads=[dnsc], writes=[dsc])
            P.op("act", lambda e: e.activation(negA[:], dsc[:, 0, :], AF.Exp), reads=[dsc], writes=[negA])
            P.op("dve", lambda e: e.tensor_scalar(negA[:], negA[:], -1.0, None, op0=ALU.mult), reads=[negA], writes=[negA])
            mrow = [P.sb("mrow%d" % i, [128, 128], F32) for i in range(2)]
            m8 = P.sb("m8", [128, NBLK, 8], F32)
            beta = P.sb("beta", [128, NBLK, 4], F32)
            nbeta = P.sb("nbeta", [128, NBLK, 4], F32)
            gall = P.sb("gall", [128, NBLK, 4], F32)
            Gc = P.sb("Gc", [128, NBLK, 4], F32)
            nG = P.sb("nG", [128, NBLK, 4], F32)
            Gt = P.sb("Gt", [128, NBLK, 4], F32)
            bEG = P.sb("bEG", [128, NBLK, 4], F32)
            eD = P.sb("eD", [128, NBLK, 4], F32)
            eGt = P.sb("eGt", [128, NBLK, 4], F32)
            if DN_STOP == "scal0":
                return
            for n in range(NBLK):
                mr = mrow[n % 2]
                P.dma("sp", mr[:], pm.h[12 * 128:13 * 128, n * 128:(n + 1) * 128], reads=[pm], writes=[mr])
                ps = pst[n % 4]
                P.op("pe", lambda e, mr=mr, ps=ps: e.transpose(ps[:, 0:128], mr[:], ident[:]), reads=[mr, ident], writes=[ps])
                P.op("act", lambda e, n=n, ps=ps: e.activation(m8[:, n, :], ps[:, 0:8], AF.Identity), reads=[ps], writes=[m8])
            if DN_STOP == "scalA":
                return
            P.op("act", lambda e: e.activation(beta[:], m8[:, :, 0:4], AF.Sigmoid), reads=[m8], writes=[beta])
            P.op("dve", lambda e: e.tensor_scalar(nbeta[:], beta[:], -1.0, None, op0=ALU.mult), reads=[beta], writes=[nbeta])
            for n in range(NBLK):
                P.op("dve", lambda e, n=n: e.tensor_tensor(gall[:, n, :], m8[:, n, 4:8], dsc[:, 1, :], ALU.add), reads=[m8, dsc], writes=[gall])
            P.op("act", lambda e: e.activation(gall[:], gall[:], AF.Exp), reads=[gall], writes=[gall])
            P.op("act", lambda e: e.activation(gall[:], gall[:], AF.Ln, bias=ones_f[:, 0:1]), reads=[gall, ones_f], writes=[gall])
            for n in range(NBLK):
                P.op("dve", lambda e, n=n: e.tensor_tensor(gall[:, n, :], gall[:, n, :], negA[:], ALU.mult), reads=[gall, negA], writes=[gall])
            if DN_STOP == "scalB":
                return
            for n in range(NBLK):
                ps = pst[n % 4]
                for d_ in range(2):
                    P.op("pe", lambda e, n=n, d_=d_, ps=ps: e.matmul(ps[:, 2 * d_:2 * d_ + 2], tri[:, d_, :], gall[:, n, 2 * d_:2 * d_ + 2], start=True, stop=True),
                         reads=[tri, gall], writes=[ps])
                P.op("pe", lambda e, n=n, ps=ps: e.matmul(ps[:, 4:8], ones_f[:], gall[:, n, :], start=True, stop=True), reads=[ones_f, gall], writes=[ps])
                P.op("dve", lambda e, n=n, ps=ps: e.tensor_copy(Gc[:, n, :], ps[:, 0:4]), reads=[ps], writes=[Gc])
                P.op("dve", lambda e, n=n, ps=ps: e.tensor_copy(Gt[:, n, :], ps[:, 4:8]), reads=[ps], writes=[Gt])
            if DN_STOP == "scalC":
                return
            P.op("dve", lambda e: e.tensor_scalar(nG[:], Gc[:], -1.0, None, op0=ALU.mult), reads=[Gc], writes=[nG])
            P.op("act", lambda e: e.activation(bEG[:], Gc[:], AF.Exp), reads=[Gc], writes=[bEG])
            P.op("dve", lambda e: e.tensor_tensor(bEG[:], bEG[:], beta[:], ALU.mult), reads=[bEG, beta], writes=[bEG])
            P.op("dve", lambda e: e.tensor_tensor(eD[:], Gt[:], Gc[:], ALU.subtract), reads=[Gt, Gc], writes=[eD])
            P.op("act", lambda e: e.activation(eD[:], eD[:], AF.Exp), reads=[eD], writes=[eD])
            P.op("act", lambda e: e.activation(eGt[:], Gt[:], AF.Exp), reads=[Gt], writes=[eGt])

            if DN_STOP == "scal":
                return
            xp = [P.sb("xp%d" % i, [128, 516], F32) for i in range(2)]
            acc = [P.sb("acc%d" % i, [128, 512], F32) for i in range(2)]
            sqd = P.sb("sqd", [128, 512], F32)
            rnd = P.sb("rnd", [128, 512], F32)

            for hh in range(2):
              with P.scope():
                  qf = P.sb("qf%d" % hh, [128, S], BF16)
                  kf = P.sb("kf%d" % hh, [128, S], BF16)
                  vf = P.sb("vf%d" % hh, [128, S], BF16)
                  nx = 0
                  for which, dst in ((0, qf), (2, kf), (4, vf)):
                      ch = which + hh
                      for (seg0, segn) in ((0, CTX), (CTX, L)):
                          Wt = min(512, segn)
                          for i in range(segn // Wt):
                              t0 = seg0 + i * Wt
                              x_, a_ = xp[nx % 2], acc[nx % 2]
                              nx += 1
                              lo = max(t0 - 2, seg0)
                              hi = min(t0 + Wt + 2, seg0 + segn)
                              P.op("pool", lambda e, x_=x_: e.memset(x_[:], 0.0), writes=[x_])
                              P.dma("sp", x_[:, lo - (t0 - 2): hi - (t0 - 2)], pm.h[ch * 128:(ch + 1) * 128, lo:hi], reads=[pm], writes=[x_])
                              P.op("dve", lambda e, x_=x_, a_=a_, ch=ch: e.tensor_scalar(a_[:, :Wt], x_[:, 0:Wt], cw[:, ch, 0:1], None, op0=ALU.mult),
                                   reads=[x_, cw], writes=[a_])
                              for tap in range(1, 5):
                                  P.op("dve", lambda e, x_=x_, a_=a_, ch=ch, tap=tap: e.scalar_tensor_tensor(
                                      a_[:, :Wt], x_[:, tap:tap + Wt], cw[:, ch, tap:tap + 1], a_[:, :Wt], op0=ALU.mult, op1=ALU.add),
                                      reads=[x_, cw, a_], writes=[a_])
                              if which == 4:
                                  P.op("act", lambda e, a_=a_, dst=dst: e.activation(dst[:, t0:t0 + Wt], a_[:, :Wt], AF.Silu), reads=[a_], writes=[dst])
                              else:
                                  P.op("act", lambda e, a_=a_: e.activation(a_[:, :Wt], a_[:, :Wt], AF.Silu), reads=[a_], writes=[a_])
                                  P.op("act", lambda e, a_=a_: e.activation(sqd[:, :Wt], a_[:, :Wt], AF.Square), reads=[a_], writes=[sqd])
                                  ps = pbig[nx % 2]
                                  P.op("pe", lambda e, ps=ps: e.matmul(ps[:, :Wt], ones_f[:], sqd[:, :Wt], start=True, stop=True), reads=[ones_f, sqd], writes=[ps])
                                  P.op("act", lambda e, ps=ps: e.activation(rnd[:, :Wt], ps[:, :Wt], AF.Sqrt, bias=eps_sb[:, 0:1]), reads=[ps, eps_sb], writes=[rnd])
                                  P.op("dve", lambda e: e.reciprocal(rnd[:, :Wt], rnd[:, :Wt]), reads=[rnd], writes=[rnd])
                                  sc_ = (128 ** -0.5) if which == 0 else 1.0
                                  P.op("dve", lambda e, a_=a_, dst=dst, sc_=sc_: e.scalar_tensor_tensor(dst[:, t0:t0 + Wt], a_[:, :Wt], sc_, rnd[:, :Wt],
                                                                                                      op0=ALU.mult, op1=ALU.mult), reads=[a_, rnd], writes=[dst])
                  chains = []
                  for d_ in range(2):
                      c = d_ * 2 + hh
                      st = dict(
                          d=d_, c=c,
                          S=P.sb("S%d" % d_, [128, 128], F32), Sb=P.sb("Sb%d" % d_, [128, 128], BF16),
                          KBG=P.sb("KBG%d" % d_, [128, 128], BF16), KD=P.sb("KD%d" % d_, [128, 128], BF16), VB=P.sb("VB%d" % d_, [128, 128], BF16),
                          Grep=P.sb("Grep%d" % d_, [128, 128], F32), t1=P.sb("t1_%d" % d_, [128, 128], F32), t2=P.sb("t2_%d" % d_, [128, 128], F32),
                          t3=P.sb("t3_%d" % d_, [128, 128], F32), dS=P.sb("dS%d" % d_, [128, 128], F32), dTi=P.sb("dTi%d" % d_, [128, 128], F32),
                          EG=P.sb("EG%d" % d_, [128, 128], F32), X=[P.sb("X%d_%d" % (d_, i), [128, 128], F32) for i in range(2)],
                          Y=[P.sb("Y%d_%d" % (d_, i), [128, 128], F32) for i in range(2)], IX=P.sb("IX%d" % d_, [128, 128], F32),
                          Pm=[P.sb("Pm%d_%d" % (d_, i), [128, 128], F32) for i in range(2)],
                          Qm=[P.sb("Qm%d_%d" % (d_, i), [128, 128], F32) for i in range(2)],
                          X1=P.sb("X1_%d" % d_, [128, 128], F32), Y1=P.sb("Y1_%d" % d_, [128, 128], F32),
                          Xo=[P.sb("Xo%d_%d" % (d_, i), [128, 128], F32) for i in range(3)],
                          Yo=[P.sb("Yo%d_%d" % (d_, i), [128, 128], F32) for i in range(3)],
                          IY=P.sb("IY%d" % d_, [128, 128], F32), U=P.sb("U%d" % d_, [128, 128], F32), bm=bmask, TTb=P.sb("TTb%d" % d_, [128, 128], BF16),
                          wT=P.sb("wT%d" % d_, [128, 128], BF16), u=P.sb("u%d" % d_, [128, 128], F32), QKT=P.sb("QKT%d" % d_, [128, 128], BF16),
                          QG=P.sb("QG%d" % d_, [128, 128], BF16), vn=P.sb("vn%d" % d_, [128, 128], BF16), ob=[P.sb("ob%d_%d" % (d_, i), [128, 128], F32) for i in range(2)],
                          ps=[banks[4 * d_ + (i_ % 4)] for i_ in range(6)],
                      )
                      P.op("pool", lambda e, st=st: e.memset(st["S"][:], 0.0), writes=[st["S"]])
                      P.op("pool", lambda e, st=st: e.memset(st["Sb"][:], 0.0), writes=[st["Sb"]])
                      if d_ == 0:
                          order = list(range(NBLK))
                      else:
                          order = list(range(CB - 1, -1, -1)) + list(range(NBLK - 1, CB - 1, -1))
                      st["order"] = order
                      chains.append(st)
                  for step in range(NBLK if not DN_STOP.startswith("scan") else int(DN_STOP[4:])):
                      for st in chains:
                          dn_block(st, st["order"][step], step, qf, kf, vf, negm, beta, nbeta, Gc, nG, bEG, eD, eGt, hh)
            if DN_STOP:
                return
            oa = [P.sb("oa%d" % i, [128, 512], F32) for i in range(2)]
            obb = [P.sb("obb%d" % i, [128, 512], F32) for i in range(2)]
            gt = [P.sb("gt%d" % i, [128, 512], F32) for i in range(2)]
            yo = [P.sb("yo%d" % i, [128, 512], BF16) for i in range(2)]
            n = 0
            for hh in range(2):
                for (seg0, segn) in ((0, CTX), (CTX, L)):
                    Wt = min(512, segn)
                    for i in range(segn // Wt):
                        t0 = seg0 + i * Wt
                        a_, b_, g_, y_ = oa[n % 2], obb[n % 2], gt[n % 2], yo[n % 2]
                        n += 1
                        P.dma("sp", a_[:, :Wt], odram[0][hh].h[:, t0:t0 + Wt], reads=[odram[0][hh]], writes=[a_])
                        P.dma("sp", b_[:, :Wt], odram[1][hh].h[:, t0:t0 + Wt], reads=[odram[1][hh]], writes=[b_])
                        P.dma("sp", g_[:, :Wt], pm.h[(6 + hh) * 128:(7 + hh) * 128, t0:t0 + Wt], reads=[pm], writes=[g_])
                        P.op("dve", lambda e, a_=a_, b_=b_: e.tensor_tensor(a_[:, :Wt], a_[:, :Wt], b_[:, :Wt], ALU.add), reads=[a_, b_], writes=[a_])
                        P.op("act", lambda e, a_=a_: e.activation(sqd[:, :Wt], a_[:, :Wt], AF.Square), reads=[a_], writes=[sqd])
                        ps = pbig[n % 2]
                        P.op("pe", lambda e, ps=ps: e.matmul(ps[:, :Wt], ones_f[:], sqd[:, :Wt], start=True, stop=True), reads=[ones_f, sqd], writes=[ps])
                        P.op("act", lambda e, ps=ps: e.activation(rnd[:, :Wt], ps[:, :Wt], AF.Sqrt, bias=eps_sb[:, 0:1], scale=1.0 / 128), reads=[ps, eps_sb], writes=[rnd])
                        P.op("dve", lambda e: e.reciprocal(rnd[:, :Wt], rnd[:, :Wt]), reads=[rnd], writes=[rnd])
                        P.op("dve", lambda e, a_=a_: e.scalar_tensor_tensor(a_[:, :Wt], a_[:, :Wt], nw_sb[:, l, 0:1], rnd[:, :Wt], op0=ALU.mult, op1=ALU.mult),
                             reads=[a_, nw_sb, rnd], writes=[a_])
                        P.op("act", lambda e, g_=g_: e.activation(g_[:, :Wt], g_[:, :Wt], AF.Silu), reads=[g_], writes=[g_])
                        P.op("pool", lambda e, a_=a_, g_=g_, y_=y_: e.tensor_tensor(y_[:, :Wt], a_[:, :Wt], g_[:, :Wt], ALU.mult), reads=[a_, g_], writes=[y_])
                        mloc_write(hh, t0, Wt, y_, lambda a, b, y_=y_: y_[:, a:b])

    DN_NOPS = int(os.environ.get("DN_NOPS", "100000"))
    dn_cnt = [0]

    def dn_block(st, n, step, qf, kf, vf, negm, beta, nbeta, Gc, nG, bEG, eD, eGt, hh, P=P):
        class _PL:
            def __getattr__(self, nm):
                return getattr(P_real, nm)

            def op(self, *a, **k):
                dn_cnt[0] += 1
                if dn_cnt[0] <= DN_NOPS:
                    P_real.op(*a, **k)

            def dma(self, *a, **k):
                dn_cnt[0] += 1
                if dn_cnt[0] <= DN_NOPS:
                    P_real.dma(*a, **k)
        P_real = P
        P = _PL()
        d_, c = st["d"], st["c"]
        ps = st["ps"]
        blk_ = slice(n * 128, (n + 1) * 128)
        Kf, Qf, Vf = kf[:, blk_], qf[:, blk_], vf[:, blk_]
        ms, mti, = (0, 1) if d_ == 0 else (3, 4)
        col = lambda t: t[:, n, c:c + 1]
        P.op("pe", lambda e: e.matmul(ps[0][:, 0:128], Kf, identb[:], start=True, stop=True), reads=[kf, identb], writes=[ps[0]])
        P.op("act", lambda e: e.activation(st["KBG"][:], ps[0][:, 0:128], AF.Identity, scale=col(bEG)), reads=[ps[0], bEG], writes=[st["KBG"]])
        P.op("dve", lambda e: e.tensor_scalar(st["KD"][:], ps[0][:, 0:128], col(eD), None, op0=ALU.mult), reads=[ps[0], eD], writes=[st["KD"]])
        P.op("pe", lambda e: e.matmul(ps[1][:, 0:128], Vf, identb[:], start=True, stop=True), reads=[vf, identb], writes=[ps[1]])
        P.op("act", lambda e: e.activation(st["VB"][:], ps[1][:, 0:128], AF.Identity, scale=col(beta)), reads=[ps[1], beta], writes=[st["VB"]])
        P.op("pool", lambda e: e.tensor_scalar(st["Grep"][:], ones_f[:], col(Gc), None, op0=ALU.mult), reads=[ones_f, Gc], writes=[st["Grep"]])
        P.op("pe", lambda e: e.matmul(ps[2][:, 0:128], st["Grep"][:], ident[:], start=True, stop=True), reads=[st["Grep"], ident], writes=[ps[2]])
        P.op("dve", lambda e: e.scalar_tensor_tensor(st["t1"][:], ps[2][:, 0:128], -1.0, negm[:, ms, :], op0=ALU.mult, op1=ALU.add), reads=[ps[2], negm], writes=[st["t1"]])
        P.op("act", lambda e: e.activation(st["dS"][:], st["t1"][:], AF.Exp, bias=col(Gc)), reads=[st["t1"], Gc], writes=[st["dS"]])
        P.op("dve", lambda e: e.tensor_tensor(st["t2"][:], ps[2][:, 0:128], negm[:, mti, :], ALU.add), reads=[ps[2], negm], writes=[st["t2"]])
        P.op("act", lambda e: e.activation(st["dTi"][:], st["t2"][:], AF.Exp, bias=col(nG)), reads=[st["t2"], nG], writes=[st["dTi"]])
        P.op("act", lambda e: e.activation(st["EG"][:], ps[2][:, 0:128], AF.Exp), reads=[ps[2]], writes=[st["EG"]])
        P.op("pe", lambda e: e.matmul(ps[3][:, 0:128], Kf, Kf, start=True, stop=True), reads=[kf], writes=[ps[3]])
        X, Y, Pm, Qm = st["X"], st["Y"], st["Pm"], st["Qm"]
        X1, Y1 = st["X1"], st["Y1"]
        bm = st["bm"]
        P.op("dve", lambda e: e.tensor_tensor(st["t3"][:], ps[3][:, 0:128], st["dS"][:], ALU.mult), reads=[ps[3], st["dS"]], writes=[st["t3"]])
        P.op("pool", lambda e: e.tensor_scalar(X1[:], st["t3"][:], col(nbeta), None, op0=ALU.mult), reads=[st["t3"], nbeta], writes=[X1])
        P.op("pe", lambda e: e.transpose(ps[4][:, 0:128], X1[:], ident[:]), reads=[X1, ident], writes=[ps[4]])
        P.op("act", lambda e: e.activation(Y1[:], ps[4][:, 0:128], AF.Identity), reads=[ps[4]], writes=[Y1])
        P.op("pool", lambda e: e.tensor_tensor(X[0][:], X1[:], bm[:, 0, :], ALU.mult), reads=[X1, bm], writes=[X[0]])
        P.op("dve", lambda e: e.tensor_tensor(Y[0][:], Y1[:], bm[:, 0, :], ALU.mult), reads=[Y1, bm], writes=[Y[0]])
        P.op("dve", lambda e: e.tensor_tensor(Pm[0][:], Y[0][:], ident[:], ALU.add), reads=[Y[0], ident], writes=[Pm[0]])
        P.op("pool", lambda e: e.tensor_tensor(Qm[0][:], X[0][:], ident[:], ALU.add), reads=[X[0], ident], writes=[Qm[0]])
        Xo, Yo = st["Xo"], st["Yo"]
        for m_ in range(3):
            mx, my = (1 + m_, 4 + m_) if d_ == 0 else (4 + m_, 1 + m_)
            P.op("pool", lambda e, m_=m_, mx=mx: e.tensor_tensor(Xo[m_][:], X1[:], bm[:, mx, :], ALU.mult), reads=[X1, bm], writes=[Xo[m_]])
            P.op("pool", lambda e, m_=m_, my=my: e.tensor_tensor(Yo[m_][:], Y1[:], bm[:, my, :], ALU.mult), reads=[Y1, bm], writes=[Yo[m_]])
        cur = 0
        pc = 0
        for lev in range(3):
            nxt = 1 - cur
            P.op("pe", lambda e, cur=cur: e.matmul(ps[3][:, 0:128], Y[cur][:], X[cur][:], start=True, stop=True), reads=[Y[cur], X[cur]], writes=[ps[3]])
            P.op("pe", lambda e, cur=cur: e.matmul(ps[4][:, 0:128], X[cur][:], Y[cur][:], start=True, stop=True), reads=[Y[cur], X[cur]], writes=[ps[4]])
            P.op("act", lambda e, nxt=nxt: e.activation(X[nxt][:], ps[3][:, 0:128], AF.Identity), reads=[ps[3]], writes=[X[nxt]])
            P.op("act", lambda e, nxt=nxt: e.activation(Y[nxt][:], ps[4][:, 0:128], AF.Identity), reads=[ps[4]], writes=[Y[nxt]])
            P.op("dve", lambda e, nxt=nxt: e.tensor_tensor(st["IX"][:], X[nxt][:], ident[:], ALU.add), reads=[X[nxt], ident], writes=[st["IX"]])
            P.op("pool", lambda e, nxt=nxt: e.tensor_tensor(st["IY"][:], Y[nxt][:], ident[:], ALU.add), reads=[Y[nxt], ident], writes=[st["IY"]])
            P.op("pe", lambda e, pc=pc: e.matmul(ps[5][:, 0:128], st["IX"][:], Pm[pc][:], start=True, stop=True), reads=[st["IX"], Pm[pc]], writes=[ps[5]])
            P.op("pe", lambda e, pc=pc: e.matmul(ps[2][:, 0:128], st["IY"][:], Qm[pc][:], start=True, stop=True), reads=[st["IY"], Qm[pc]], writes=[ps[2]])
            P.op("dve", lambda e, pc=pc: e.tensor_copy(Pm[1 - pc][:], ps[5][:, 0:128]), reads=[ps[5]], writes=[Pm[1 - pc]])
            P.op("act", lambda e, pc=pc: e.activation(Qm[1 - pc][:], ps[2][:, 0:128], AF.Identity), reads=[ps[2]], writes=[Qm[1 - pc]])
            pc = 1 - pc
            cur = nxt
        for m_ in range(3):
            lastm = (m_ == 2)
            P.op("pe", lambda e, m_=m_, pc=pc: e.matmul(ps[3][:, 0:128], Yo[m_][:], Qm[pc][:], start=True, stop=True), reads=[Yo[m_], Qm[pc]], writes=[ps[3]])
            P.op("act", lambda e: e.activation(st["U"][:], ps[3][:, 0:128], AF.Identity), reads=[ps[3]], writes=[st["U"]])
            P.op("pe", lambda e, pc=pc: e.matmul(ps[5][:, 0:128], ident[:], Pm[pc][:], start=True, stop=False), reads=[ident, Pm[pc]], writes=[ps[5]])
            P.op("pe", lambda e, pc=pc: e.matmul(ps[5][:, 0:128], st["U"][:], Pm[pc][:], start=False, stop=True), reads=[st["U"], Pm[pc]], writes=[ps[5]])
            if lastm:
                P.op("dve", lambda e: e.tensor_copy(st["TTb"][:], ps[5][:, 0:128]), reads=[ps[5]], writes=[st["TTb"]])
            else:
                P.op("pe", lambda e, pc=pc: e.matmul(ps[2][:, 0:128], ident[:], Qm[pc][:], start=True, stop=False), reads=[ident, Qm[pc]], writes=[ps[2]])
                P.op("pe", lambda e, pc=pc: e.matmul(ps[2][:, 0:128], Pm[pc][:], st["U"][:], start=False, stop=True), reads=[st["U"], Pm[pc]], writes=[ps[2]])
                P.op("dve", lambda e, pc=pc: e.tensor_copy(Pm[1 - pc][:], ps[5][:, 0:128]), reads=[ps[5]], writes=[Pm[1 - pc]])
                P.op("act", lambda e, pc=pc: e.activation(Qm[1 - pc][:], ps[2][:, 0:128], AF.Identity), reads=[ps[2]], writes=[Qm[1 - pc]])
                pc = 1 - pc
        P.op("pe", lambda e: e.matmul(ps[0][:, 0:128], st["KBG"][:], st["TTb"][:], start=True, stop=True), reads=[st["KBG"], st["TTb"]], writes=[ps[0]])
        P.op("act", lambda e: e.activation(st["wT"][:], ps[0][:, 0:128], AF.Identity), reads=[ps[0]], writes=[st["wT"]])
        P.op("pe", lambda e: e.matmul(ps[1][:, 0:128], st["TTb"][:], st["VB"][:], start=True, stop=True), reads=[st["VB"], st["TTb"]], writes=[ps[1]])
        P.op("act", lambda e: e.activation(st["u"][:], ps[1][:, 0:128], AF.Identity), reads=[ps[1]], writes=[st["u"]])
        P.op("pe", lambda e: e.matmul(ps[2][:, 0:128], Kf, Qf, start=True, stop=True), reads=[kf, qf, st["EG"], st["t1"], st["t2"]], writes=[ps[2]])
        P.op("dve", lambda e: e.tensor_tensor(st["QKT"][:], ps[2][:, 0:128], st["dTi"][:], ALU.mult), reads=[ps[2], st["dTi"]], writes=[st["QKT"]])
        P.op("pool", lambda e: e.tensor_tensor(st["QG"][:], Qf, st["EG"][:], ALU.mult), reads=[qf, st["EG"]], writes=[st["QG"]])
        P.op("pe", lambda e: e.matmul(ps[3][:, 0:128], st["wT"][:], st["Sb"][:], start=True, stop=True), reads=[st["wT"], st["Sb"]], writes=[ps[3]])
        P.op("dve", lambda e: e.tensor_tensor(st["vn"][:], st["u"][:], ps[3][:, 0:128], ALU.subtract), reads=[st["u"], ps[3]], writes=[st["vn"]])
        P.op("pe", lambda e: e.matmul(ps[4][:, 0:128], st["Sb"][:], st["QG"][:], start=True, stop=False), reads=[st["Sb"], st["QG"]], writes=[ps[4]])
        P.op("pe", lambda e: e.matmul(ps[4][:, 0:128], st["vn"][:], st["QKT"][:], start=False, stop=True), reads=[st["vn"], st["QKT"]], writes=[ps[4]])
        ob = st["ob"][step % 2]
        P.op("act", lambda e: e.activation(ob[:], ps[4][:, 0:128], AF.Identity), reads=[ps[4]], writes=[ob])
        od = odram[d_][hh]
        P.dma("sp", od.h[:, blk_], ob[:], reads=[ob], writes=[od])
        P.op("pe", lambda e: e.matmul(ps[5][:, 0:128], st["KD"][:], st["vn"][:], start=True, stop=True), reads=[st["KD"], st["vn"]], writes=[ps[5]])
        P.op("dve", lambda e: e.scalar_tensor_tensor(st["S"][:], st["S"][:], col(eGt), ps[5][:, 0:128], op0=ALU.mult, op1=ALU.add),
             reads=[st["S"], eGt, ps[5]], writes=[st["S"]])
        P.op("act", lambda e: e.activation(st["Sb"][:], st["S"][:], AF.Identity), reads=[st["S"]], writes=[st["Sb"]])

    def dump(name, src, shape, dt):
        t = dbg_out(name, shape, dt)
        if t is not None:
            P.dma("sp", t.h.ap(), src.h.ap(), reads=[src], writes=[t])

    prep_layer(0)
    cosL.emit_casts()
    sinL.emit_casts()
    for ch in cosL.chunks + sinL.chunks:
        ch[2].w = ("WCOPY", P.cnt["WCOPY"])
    cosL.emit_gathers()
    sinL.emit_gathers()
    for l in range(DEPTH):
        P0(l)
        P1(l)
        for kc in range(KC):
            P.allgather(h1_loc, h1_all, GRP, h1_loc.h[kc], h1_all.h[kc])
        if stop_after == ("P1", l):
            break
        P2(l)
        if debug and l == 0:
            dump("pm", pm, [13 * 128, S], F32)
        if stop_after == ("P2", l):
            break
        P3c(l)
        if stop_after == ("P3c", l):
            break
        P3b(l)
        if stop_after == ("P3b", l):
            break
        if l + 1 < DEPTH:
            prep_casts(l + 1)
        P3a(l)
        if stop_after == ("P3a", l):
            break
        if debug and l == 0:
            dump("mloc", m_loc, [4, 4, 128, TOK], BF16)
        for i_ in range(4):
            for j_ in range(4):
                P.allgather(m_loc, m_all, GRP, m_loc.h[i_, j_], m_all.h[j_, i_])
        if l + 1 < DEPTH:
            prep_gathers(l + 1)
        if stop_after == ("P3", l):
            break
        P4(l, l == DEPTH - 1)
        if debug and l == 0:
            dump("x1", xres, [D, TOK], F32)
    finals = [out_d] + list(dbg.values())
    nc = P.finish(finals)
    return nc, list(dbg.keys())


_CACHE = {}


def run(cfg, inputs, debug=False, nlayers=None, stop_after=None):
    key = (cfg.D, cfg.L, cfg.DFF, cfg.DEPTH, debug, nlayers, stop_after)
    if key not in _CACHE:
        _CACHE[key] = build(cfg, debug=debug, nlayers=nlayers, stop_after=stop_after)
    nc, dbgnames = _CACHE[key]
    maps = make_in_maps(cfg, inputs)
    res = run_bass_kernel_spmd(nc, maps, core_ids=list(range(8)))
    B = 2
    out = np.zeros((B, cfg.L, cfg.D), np.float32)
    for c in range(8):
        b, j = c // 4, c % 4
        out[b, j * cfg.NL:(j + 1) * cfg.NL, :] = res.results[c]["out"].T
    return out, res


def kernel(**inputs):
    cfg = Cfg()
    out, _ = run(cfg, inputs)
    return out
```
